# Optimizing a Trainium2 kernel written in Bass

```python
import jax
import jax.numpy as jnp
from jax import lax
import numpy as np

D_MODEL = 1024
BATCH = 4
SEQ = 8192
DEPTH = 4

GRID_W = 64
CTX_LEN = 256
HEAD_DIM = 64
ROPE_BASE = 10000.0
NORM_EPS = 1e-6
NEG_INF = -1e30
Q_BLOCK = 128

A_HEADS = 8
A_KV_HEADS = 2
A_WINDOW = 128
B_HEADS = 8
B_NOPE = 64
B_ROPE = 32
B_V = 64
B_Q_RANK = 768
B_KV_RANK = 256
C_HEADS = 16
NA_ROWS = 8
NA_COLS = 16

MIX_WIDTH = A_HEADS * HEAD_DIM + B_HEADS * B_V
EVEN_SPLIT = (A_HEADS * HEAD_DIM, A_KV_HEADS * HEAD_DIM, A_KV_HEADS * HEAD_DIM, B_Q_RANK, B_KV_RANK, B_ROPE)
EVEN_IN = sum(EVEN_SPLIT)
ODD_WIDTH = C_HEADS * HEAD_DIM
ODD_IN = 3 * ODD_WIDTH

N_EXPERTS = 32
TOP_K = 4
D_EXPERT = 1024
SWIGLU_LIMIT = 7.0
SWIGLU_ALPHA = 1.702
MOE_BLOCK = 256

N_EVEN = (DEPTH + 1) // 2
N_ODD = DEPTH // 2

kernel_name = 'hybrid_dit_swa_mla_natten_moe'


def _rmsnorm(x, g):
    x32 = x.astype(jnp.float32)
    y = x32 * lax.rsqrt(jnp.mean(x32 * x32, axis=-1, keepdims=True) + NORM_EPS)
    return (y * g.astype(jnp.float32)).astype(x.dtype)


def _modulate(x, g, shift, scale):
    return _rmsnorm(x, g) * (1.0 + scale) + shift


def _split_cols(p, sizes):
    return jnp.split(p, np.cumsum(sizes)[:-1].tolist(), axis=-1)


def _rope_1d(x, pos):
    n = x.shape[-1]
    half = n // 2
    inv_freq = ROPE_BASE ** (-(jnp.arange(half, dtype=jnp.float32) / half))
    ang = pos.astype(jnp.float32)[:, None] * inv_freq
    shape = (ang.shape[0],) + (1,) * (x.ndim - 3) + (half,)
    cos = jnp.cos(ang).reshape(shape).astype(x.dtype)
    sin = jnp.sin(ang).reshape(shape).astype(x.dtype)
    x1, x2 = x[..., :half], x[..., half:]
    return jnp.concatenate([x1 * cos - x2 * sin, x2 * cos + x1 * sin], axis=-1)


def _rope2d(x, rows, cols):
    h = x.shape[-1] // 2
    return jnp.concatenate([_rope_1d(x[..., :h], rows), _rope_1d(x[..., h:], cols)], axis=-1)


def _attend_dense(q, k, v, sink):
    bsz, n, h, d = q.shape
    kv = k.shape[2]
    m = k.shape[1]
    qg = q.reshape(bsz, n, kv, h // kv, d)
    s = jnp.einsum('bqkgd,bmkd->bkgqm', qg, k).astype(jnp.float32) * (d ** -0.5)
    if sink is not None:
        sk = jnp.broadcast_to(sink.astype(jnp.float32).reshape(kv, h // kv, 1, 1), s.shape[:-1] + (1,))
        s = jnp.concatenate([s, sk], axis=-1)
    p = jax.nn.softmax(s, axis=-1).astype(q.dtype)[..., :m]
    return jnp.einsum('bkgqm,bmkd->bqkgd', p, v).reshape(bsz, n, h * d)


def _window_attn_latent(q, k, v, kc, vc, sink):
    bsz, s_len, h, d = q.shape
    kv = k.shape[2]
    g = h // kv
    w = A_WINDOW
    nb = s_len // w
    n_ctx = kc.shape[1]
    qb = jnp.swapaxes(q.reshape(bsz, nb, w, kv, g, d), 0, 1)
    pad = ((0, 0), (w, w), (0, 0), (0, 0))
    kp = jnp.pad(k, pad)
    vp = jnp.pad(v, pad)
    sink_l = sink.astype(jnp.float32).reshape(kv, g, 1, 1)
    band = jnp.abs(jnp.arange(w)[:, None] + w - jnp.arange(3 * w)[None, :]) <= w
    scale = d ** -0.5

    def block(args):
        n, qn = args
        kn = lax.dynamic_slice_in_dim(kp, n * w, 3 * w, axis=1)
        vn = lax.dynamic_slice_in_dim(vp, n * w, 3 * w, axis=1)
        kpos = n * w - w + jnp.arange(3 * w)
        mask = band & ((kpos >= 0) & (kpos < s_len))[None, :]
        sw = jnp.einsum('bqkgd,bskd->bkgqs', qn, kn).astype(jnp.float32) * scale
        sw = jnp.where(mask, sw, NEG_INF)
        sc = jnp.einsum('bqkgd,bckd->bkgqc', qn, kc).astype(jnp.float32) * scale
        sk = jnp.broadcast_to(sink_l, sw.shape[:-1] + (1,))
        p = jax.nn.softmax(jnp.concatenate([sw, sc, sk], axis=-1), axis=-1).astype(q.dtype)
        pw = p[..., :3 * w]
        pc = p[..., 3 * w:3 * w + n_ctx]
        return (jnp.einsum('bkgqs,bskd->bqkgd', pw, vn)
                + jnp.einsum('bkgqc,bckd->bqkgd', pc, vc))

    out = lax.map(block, (jnp.arange(nb, dtype=jnp.int32), qb))
    return jnp.swapaxes(out, 0, 1).reshape(bsz, s_len, h * d)


def _mla_attend(qn, qp, kn, kpe, v):
    s = (jnp.einsum('bqhd,bkhd->bhqk', qn, kn).astype(jnp.float32)
         + jnp.einsum('bqhr,bkr->bhqk', qp, kpe).astype(jnp.float32)) * ((B_NOPE + B_ROPE) ** -0.5)
    p = jax.nn.softmax(s, axis=-1).astype(v.dtype)
    return jnp.einsum('bhqk,bkhd->bqhd', p, v)


def _mla_latent(qn, qp, kn, kpe, v, kn_c, kpe_c, v_c):
    bsz, s_len, h, _ = qn.shape
    nb = s_len // Q_BLOCK
    kn_all = jnp.concatenate([kn, kn_c], axis=1)
    kpe_all = jnp.concatenate([kpe, kpe_c], axis=1)
    v_all = jnp.concatenate([v, v_c], axis=1)
    qn_b = jnp.swapaxes(qn.reshape(bsz, nb, Q_BLOCK, h, -1), 0, 1)
    qp_b = jnp.swapaxes(qp.reshape(bsz, nb, Q_BLOCK, h, -1), 0, 1)
    out = lax.map(lambda a: _mla_attend(a[0], a[1], kn_all, kpe_all, v_all), (qn_b, qp_b))
    return jnp.swapaxes(out, 0, 1).reshape(bsz, s_len, h * B_V)


def _even_heads(h, w_in, q_norm_g, w_uq, kv_norm_g, w_ukv, rows, cols):
    bsz, n, _ = h.shape
    qa, ka, va, cq, ckv, kpe = _split_cols(h @ w_in, EVEN_SPLIT)
    qa = qa.reshape(bsz, n, A_HEADS, HEAD_DIM)
    ka = ka.reshape(bsz, n, A_KV_HEADS, HEAD_DIM)
    va = va.reshape(bsz, n, A_KV_HEADS, HEAD_DIM)
    qb = (_rmsnorm(cq, q_norm_g) @ w_uq).reshape(bsz, n, B_HEADS, B_NOPE + B_ROPE)
    qn, qp = qb[..., :B_NOPE], qb[..., B_NOPE:]
    kvb = (_rmsnorm(ckv, kv_norm_g) @ w_ukv).reshape(bsz, n, B_HEADS, B_NOPE + B_V)
    kn, vb = kvb[..., :B_NOPE], kvb[..., B_NOPE:]
    if rows is not None:
        qa = _rope2d(qa, rows, cols)
        ka = _rope2d(ka, rows, cols)
        qp = _rope2d(qp, rows, cols)
        kpe = _rope2d(kpe, rows, cols)
    return qa, ka, va, qn, qp, kn, kpe, vb


def _even_mixer(hl, hc, rows, cols, w_in, sink, q_norm_g, w_uq, kv_norm_g, w_ukv, w_out, need_ctx):
    qa, ka, va, qn, qp, kn, kpe, vb = _even_heads(hl, w_in, q_norm_g, w_uq, kv_norm_g, w_ukv, rows, cols)
    qa_c, ka_c, va_c, qn_c, qp_c, kn_c, kpe_c, vb_c = _even_heads(hc, w_in, q_norm_g, w_uq, kv_norm_g, w_ukv, None, None)
    oa = _window_attn_latent(qa, ka, va, ka_c, va_c, sink)
    ob = _mla_latent(qn, qp, kn, kpe, vb, kn_c, kpe_c, vb_c)
    out_l = jnp.concatenate([oa, ob], axis=-1) @ w_out
    if not need_ctx:
        return out_l, None
    oa_c = _attend_dense(qa_c, ka_c, va_c, sink)
    ob_c = _mla_attend(qn_c, qp_c, kn_c, kpe_c, vb_c).reshape(hc.shape[0], hc.shape[1], B_HEADS * B_V)
    out_c = jnp.concatenate([oa_c, ob_c], axis=-1) @ w_out
    return out_l, out_c


def _na_latent(q, k, v, kc, vc, rpb):
    bsz, s_len, h, d = q.shape
    n_rows = s_len // GRID_W
    kh = min(NA_ROWS, n_rows)
    kw = NA_COLS
    scale = d ** -0.5
    qg = q.reshape(bsz, n_rows, GRID_W, h, d)
    kg = k.reshape(bsz, n_rows, GRID_W, h, d)
    vg = v.reshape(bsz, n_rows, GRID_W, h, d)
    col = jnp.arange(GRID_W)
    col_idx = jnp.clip(col - kw // 2, 0, GRID_W - kw)[:, None] + jnp.arange(kw)[None, :]
    dc = col_idx - col[:, None] + (NA_COLS - 1)

    def row(args):
        r, qr = args
        r0 = jnp.clip(r - kh // 2, 0, n_rows - kh)
        kb = lax.dynamic_slice_in_dim(kg, r0, kh, axis=1)[:, :, col_idx]
        vb = lax.dynamic_slice_in_dim(vg, r0, kh, axis=1)[:, :, col_idx]
        dr = r0 + jnp.arange(kh) - r + (NA_ROWS - 1)
        bias = jnp.transpose(rpb[:, dr[:, None, None], dc[None, :, :]], (0, 2, 1, 3))
        sn = jnp.einsum('bqhd,biqjhd->bhqij', qr, kb).astype(jnp.float32) * scale + bias.astype(jnp.float32)
        sn = sn.reshape(bsz, h, GRID_W, kh * kw)
        sc = jnp.einsum('bqhd,bchd->bhqc', qr, kc).astype(jnp.float32) * scale
        p = jax.nn.softmax(jnp.concatenate([sn, sc], axis=-1), axis=-1).astype(q.dtype)
        pn = p[..., :kh * kw].reshape(bsz, h, GRID_W, kh, kw)
        pc = p[..., kh * kw:]
        return (jnp.einsum('bhqij,biqjhd->bqhd', pn, vb)
                + jnp.einsum('bhqc,bchd->bqhd', pc, vc))

    out = lax.map(row, (jnp.arange(n_rows, dtype=jnp.int32), jnp.swapaxes(qg, 0, 1)))
    return jnp.swapaxes(out, 0, 1).reshape(bsz, s_len, h * d)


def _odd_heads(h, w_in):
    bsz, n, _ = h.shape
    q, k, v = jnp.split(h @ w_in, 3, axis=-1)
    shp = (bsz, n, C_HEADS, HEAD_DIM)
    return q.reshape(shp), k.reshape(shp), v.reshape(shp)


def _odd_mixer(hl, hc, w_in, rpb, w_out, need_ctx):
    q, k, v = _odd_heads(hl, w_in)
    q_c, k_c, v_c = _odd_heads(hc, w_in)
    out_l = _na_latent(q, k, v, k_c, v_c, rpb) @ w_out
    if not need_ctx:
        return out_l, None
    return out_l, _attend_dense(q_c, k_c, v_c, None) @ w_out


def _clamped_swiglu(u):
    glu, lin = u[..., :D_EXPERT], u[..., D_EXPERT:]
    glu = jnp.minimum(glu, SWIGLU_LIMIT)
    lin = jnp.clip(lin, -SWIGLU_LIMIT, SWIGLU_LIMIT)
    return glu * jax.nn.sigmoid(SWIGLU_ALPHA * glu) * (lin + 1.0)


def _moe(h, router_w, router_b, w1, b1, w2, b2):
    n_tok, d = h.shape
    logits = (h @ router_w + router_b).astype(jnp.float32)
    top_val, top_exp = lax.top_k(logits, TOP_K)
    gates = jax.nn.softmax(top_val, axis=-1)
    n_asg = n_tok * TOP_K
    flat_e = top_exp.reshape(n_asg)
    order = jnp.argsort(flat_e)
    sorted_e = flat_e[order]
    counts = jnp.zeros((N_EXPERTS,), jnp.int32).at[flat_e].add(1)
    padded = (counts + MOE_BLOCK - 1) // MOE_BLOCK * MOE_BLOCK
    pad_end = jnp.cumsum(padded)
    pad_start = pad_end - padded
    grp_start = jnp.cumsum(counts) - counts
    dest = pad_start[sorted_e] + jnp.arange(n_asg, dtype=jnp.int32) - grp_start[sorted_e]
    n_blk = (n_asg + N_EXPERTS * (MOE_BLOCK - 1) + MOE_BLOCK - 1) // MOE_BLOCK
    n_slot = n_blk * MOE_BLOCK
    buf_tok = jnp.full((n_slot,), n_tok, jnp.int32).at[dest].set((order // TOP_K).astype(jnp.int32))
    buf_gate = jnp.zeros((n_slot,), jnp.float32).at[dest].set(gates.reshape(n_asg)[order])
    blk_exp = jnp.minimum(jnp.searchsorted(pad_end, jnp.arange(n_blk, dtype=jnp.int32) * MOE_BLOCK, side='right'), N_EXPERTS - 1)
    h_pad = jnp.concatenate([h, jnp.zeros((1, d), h.dtype)], axis=0)

    def block(args):
        tok, gate, e = args
        u = h_pad[tok] @ w1[e] + b1[e]
        y = _clamped_swiglu(u) @ w2[e] + b2[e]
        return y * gate[:, None].astype(y.dtype)

    ys = lax.map(block, (buf_tok.reshape(n_blk, MOE_BLOCK), buf_gate.reshape(n_blk, MOE_BLOCK), blk_exp))
    return jnp.zeros((n_tok + 1, d), h.dtype).at[buf_tok].add(ys.reshape(n_slot, d))[:n_tok]


def setup_inputs(seed: int = 0) -> dict:
    key = jax.random.key(seed)
    ks = jax.random.split(key, 25)
    d = D_MODEL

    def nrm(k, shape, std):
        return jax.random.normal(k, shape, jnp.float32) * std

    return {
        'x': nrm(ks[0], (BATCH, SEQ, d), 1.0),
        'c': nrm(ks[1], (BATCH, d), 1.0),
        'ctx': nrm(ks[2], (BATCH, CTX_LEN, d), 1.0),
        'c_ctx': nrm(ks[3], (d,), 1.0),
        'ada_w': nrm(ks[4], (DEPTH, d, 6 * d), 0.5 * d ** -0.5),
        'ada_b': nrm(ks[5], (DEPTH, 6 * d), 0.02),
        'norm1_g': 1.0 + nrm(ks[6], (DEPTH, d), 0.05),
        'norm2_g': 1.0 + nrm(ks[7], (DEPTH, d), 0.05),
        'ev_w_in': nrm(ks[8], (N_EVEN, d, EVEN_IN), d ** -0.5),
        'ev_sink': nrm(ks[9], (N_EVEN, A_HEADS), 1.0),
        'ev_q_norm_g': 1.0 + nrm(ks[10], (N_EVEN, B_Q_RANK), 0.05),
        'ev_w_uq': nrm(ks[11], (N_EVEN, B_Q_RANK, B_HEADS * (B_NOPE + B_ROPE)), B_Q_RANK ** -0.5),
        'ev_kv_norm_g': 1.0 + nrm(ks[12], (N_EVEN, B_KV_RANK), 0.05),
        'ev_w_ukv': nrm(ks[13], (N_EVEN, B_KV_RANK, B_HEADS * (B_NOPE + B_V)), B_KV_RANK ** -0.5),
        'ev_w_out': nrm(ks[14], (N_EVEN, MIX_WIDTH, d), MIX_WIDTH ** -0.5),
        'od_w_in': nrm(ks[15], (N_ODD, d, ODD_IN), d ** -0.5),
        'od_rpb': nrm(ks[16], (N_ODD, C_HEADS, 2 * NA_ROWS - 1, 2 * NA_COLS - 1), 0.5),
        'od_w_out': nrm(ks[17], (N_ODD, ODD_WIDTH, d), ODD_WIDTH ** -0.5),
        'router_w': nrm(ks[18], (DEPTH, d, N_EXPERTS), d ** -0.5),
        'router_b': nrm(ks[19], (DEPTH, N_EXPERTS), 0.01),
        'exp_w1': nrm(ks[20], (DEPTH, N_EXPERTS, d, 2 * D_EXPERT), d ** -0.5),
        'exp_b1': nrm(ks[21], (DEPTH, N_EXPERTS, 2 * D_EXPERT), 0.02),
        'exp_w2': nrm(ks[22], (DEPTH, N_EXPERTS, D_EXPERT, d), D_EXPERT ** -0.5),
        'exp_b2': nrm(ks[23], (DEPTH, N_EXPERTS, d), 0.02),
        'final_g': 1.0 + nrm(ks[24], (d,), 0.05),
    }


def reference(x, c, ctx, c_ctx, ada_w, ada_b, norm1_g, norm2_g, ev_w_in, ev_sink, ev_q_norm_g, ev_w_uq,
              ev_kv_norm_g, ev_w_ukv, ev_w_out, od_w_in, od_rpb, od_w_out, router_w, router_b,
              exp_w1, exp_b1, exp_w2, exp_b2, final_g):
    bsz, seq, d = x.shape
    n_ctx = ctx.shape[1]
    t = jnp.arange(seq, dtype=jnp.int32)
    rows, cols = t // GRID_W, t % GRID_W
    silu_c = jax.nn.silu(c)
    silu_cc = jax.nn.silu(c_ctx)[None, :]
    xl, xc = x, ctx
    for layer in range(DEPTH):
        last = layer == DEPTH - 1
        i = layer // 2
        mod_l = jnp.split((silu_c @ ada_w[layer] + ada_b[layer])[:, None, :], 6, axis=-1)
        mod_c = jnp.split((silu_cc @ ada_w[layer] + ada_b[layer])[:, None, :], 6, axis=-1)
        hl = _modulate(xl, norm1_g[layer], mod_l[0], mod_l[1])
        hc = _modulate(xc, norm1_g[layer], mod_c[0], mod_c[1])
        if layer % 2 == 0:
            ml, mc = _even_mixer(hl, hc, rows, cols, ev_w_in[i], ev_sink[i], ev_q_norm_g[i], ev_w_uq[i],
                                 ev_kv_norm_g[i], ev_w_ukv[i], ev_w_out[i], not last)
        else:
            ml, mc = _odd_mixer(hl, hc, od_w_in[i], od_rpb[i], od_w_out[i], not last)
        xl = xl + mod_l[2] * ml
        hl = _modulate(xl, norm2_g[layer], mod_l[3], mod_l[4]).reshape(bsz * seq, d)
        if last:
            y = _moe(hl, router_w[layer], router_b[layer], exp_w1[layer], exp_b1[layer], exp_w2[layer], exp_b2[layer])
            xl = xl + mod_l[5] * y.reshape(bsz, seq, d)
        else:
            xc = xc + mod_c[2] * mc
            hc = _modulate(xc, norm2_g[layer], mod_c[3], mod_c[4]).reshape(bsz * n_ctx, d)
            y = _moe(jnp.concatenate([hl, hc], axis=0), router_w[layer], router_b[layer], exp_w1[layer],
                     exp_b1[layer], exp_w2[layer], exp_b2[layer])
            xl = xl + mod_l[5] * y[:bsz * seq].reshape(bsz, seq, d)
            xc = xc + mod_c[5] * y[bsz * seq:].reshape(bsz, n_ctx, d)
    return _rmsnorm(xl, final_g)
```

```python
import sys, time, contextlib, os
import numpy as np
import concourse.bass as bass
import concourse.mybir as mybir
from concourse.bass_utils import run_bass_kernel_spmd

F32 = mybir.dt.float32
BF16 = mybir.dt.bfloat16
I32 = mybir.dt.int32
U32 = mybir.dt.uint32
ALU = mybir.AluOpType
AF = mybir.ActivationFunctionType
AX = mybir.AxisListType

EPOCH = 30000
NDMASEM = 16
import os
NDMA = {"sp": int(os.environ.get("FW_SP", 6)), "pool": int(os.environ.get("FW_POOL", 3)), "act": 4, "pe": 1, "dve": 1}


def _kr(key):
    if isinstance(key, tuple):
        return key[0], (key[1] if len(key) == 2 else key[1:])
    return key, None


class Prog:
    ENGS = ('pe', 'dve', 'act', 'pool', 'sp')

    def __init__(self, nc):
        self.nc = nc
        self.ops = []
        self.state = {}
        self.dma_hist = {e: [] for e in self.ENGS}

    def _conf(self, st, region):
        if region is None:
            return list(st.keys())
        return [r for r in st.keys() if r is None or r == region]

    def op(self, eng, fn, reads=(), writes=(), dma=False):
        idx = len(self.ops)
        deps = set()
        for key in reads:
            buf, region = _kr(key)
            st = self.state.setdefault(buf, {'W': {}, 'R': {}})
            for r in self._conf(st['W'], region):
                deps.add(st['W'][r])
        for key in writes:
            buf, region = _kr(key)
            st = self.state.setdefault(buf, {'W': {}, 'R': {}})
            for r in self._conf(st['W'], region):
                deps.add(st['W'][r])
            for r in self._conf(st['R'], region):
                deps.update(st['R'][r])
        for key in reads:
            buf, region = _kr(key)
            st = self.state[buf]
            lst = st['R'].setdefault(region, [])
            if dma:
                mine = [j for j in lst if self.ops[j]['dma'] and self.ops[j]['eng'] == eng]
                if len(mine) >= NDMA[eng]:
                    lst.remove(mine[0])
            else:
                for j in lst:
                    if (not self.ops[j]['dma']) and self.ops[j]['eng'] == eng:
                        lst.remove(j)
                        break
            lst.append(idx)
        for key in writes:
            buf, region = _kr(key)
            st = self.state[buf]
            if region is None:
                st['W'] = {None: idx}
                st['R'] = {}
            else:
                st['W'][region] = idx
                st['R'].pop(region, None)
        if dma:
            h = self.dma_hist[eng]
            if len(h) >= NDMA[eng]:
                deps.add(h[len(h) - NDMA[eng]])
            h.append(idx)
        deps.discard(idx)
        self.ops.append(dict(eng=eng, fn=fn, deps=deps, dma=dma))
        return idx

    def emit(self):
        nc = self.nc
        ops = self.ops
        n = len(ops)
        for o in ops:
            o['deps'] = {d for d in o['deps']
                         if not (o['eng'] == 'pe' and ops[d]['eng'] == 'pe' and not ops[d]['dma'])}
        needed = [False] * n
        for o in ops:
            for d in o['deps']:
                needed[d] = True
        cnt = {e: 0 for e in self.ENGS}
        dcnt = {e: 0 for e in self.ENGS}
        comp = [None] * n
        for i, o in enumerate(ops):
            e = o['eng']
            if o['dma']:
                k = dcnt[e]
                dcnt[e] += 1
                comp[i] = ('d', e, k % NDMA[e], 16 * (k // NDMA[e] + 1))
            elif needed[i]:
                k = cnt[e]
                cnt[e] += 1
                comp[i] = ('c', e, k // EPOCH, k % EPOCH + 1)
        nep = {e: max(1, (cnt[e] + EPOCH - 1) // EPOCH) for e in self.ENGS}
        import contextlib
        with contextlib.ExitStack() as es:
            csem = {e: [es.enter_context(nc.semaphore(f"c_{e}_{j}")) for j in range(nep[e])]
                    for e in self.ENGS}
            dsem = {e: [es.enter_context(nc.semaphore(f"d_{e}_{j}")) for j in range(NDMA[e])]
                    for e in self.ENGS if dcnt[e] > 0}
            block = es.enter_context(nc.Block())
            per_eng = {e: [i for i in range(n) if ops[i]['eng'] == e] for e in self.ENGS}

            def body(ename):
                def run(eng):
                    waited = {}
                    for i in per_eng[ename]:
                        o = ops[i]
                        need = {}
                        for d in o['deps']:
                            kind, e2, a, b = comp[d]
                            key = (kind, e2, a)
                            if kind == 'c':
                                pass
                            need[key] = max(need.get(key, 0), b)
                        for key, v in sorted(need.items()):
                            if waited.get(key, 0) >= v:
                                continue
                            kind, e2, a = key
                            if kind == 'c':
                                later = [k for k in waited if k[0] == 'c' and k[1] == e2 and k[2] > a]
                                if later:
                                    continue
                                eng.wait_ge(csem[e2][a], v)
                            else:
                                eng.wait_ge(dsem[e2][a], v)
                            waited[key] = v
                        ins = o['fn'](eng)
                        c = comp[i]
                        if c is not None:
                            if c[0] == 'c':
                                ins.then_inc(csem[c[1]][c[2]], 1)
                            else:
                                ins.then_inc(dsem[c[1]][c[2]], 16)
                    if ename in dsem:
                        k = dcnt[ename]
                        for j in range(NDMA[ename]):
                            nj = (k - j + NDMA[ename] - 1) // NDMA[ename]
                            if nj > 0:
                                eng.wait_ge(dsem[ename][j], 16 * nj)
                return run

            if per_eng['pe']:
                block.tensor(body('pe'))
            if per_eng['dve']:
                block.vector(body('dve'))
            if per_eng['act']:
                block.scalar(body('act'))
            if per_eng['pool']:
                block.gpsimd(body('pool'))
            if per_eng['sp']:
                block.sync(body('sp'))
        return dict(n_ops=n, cnt=cnt, dcnt=dcnt)


D = 1024
NQ = 4096
NO = 4096
NCX = 256
NT_OWN = NQ // 128
EPS = 1e-6
NKA = 128 + NQ + 128 + NCX
NKM = NQ + NO + NCX
NQA = NQ + NCX
W_IN_EXT = 1824 + 512 + 128 + 32


class KB:
    def __init__(self, nc):
        self.nc = nc
        self.P = Prog(nc)
        self.es = contextlib.ExitStack()
        self.uid = 0

    def sb(self, name, shape, dt):
        t = self.es.enter_context(self.nc.sbuf_tensor(name, shape, dt))
        t.key = name
        return t

    def ps(self, name, shape, dt):
        t = self.es.enter_context(self.nc.psum_tensor(name, shape, dt))
        t.key = name
        return t

    def dram(self, name, shape, dt, kind="Internal"):
        return self.nc.dram_tensor(name, shape, dt, kind=kind).ap()

    def dma(self, q, out, in_, reads, writes):
        return self.P.op(q, lambda e: e.dma_start(out=out, in_=in_), reads, writes, dma=True)

    def mm(self, out, lhsT, rhs, start, stop, reads, writes):
        return self.P.op('pe', lambda e: e.matmul(out, lhsT=lhsT, rhs=rhs, start=start, stop=stop), reads, writes)

    def tr(self, out, in_, ident, reads, writes):
        return self.P.op('pe', lambda e: e.transpose(out=out, in_=in_, identity=ident), reads, writes)

    def act(self, out, in_, func, reads, writes, scale=1.0, bias=0.0, accum=None, eng='act'):
        if accum is None:
            return self.P.op(eng, lambda e: e.activation(out=out, in_=in_, func=func, bias=bias, scale=scale), reads, writes)
        return self.P.op(eng, lambda e: e.activation(out=out, in_=in_, func=func, bias=bias, scale=scale, accum_out=accum), reads, writes)

    def copy(self, eng, out, in_, reads, writes):
        if eng == 'act':
            return self.P.op(eng, lambda e: e.copy(out=out, in_=in_), reads, writes)
        return self.P.op(eng, lambda e: e.tensor_copy(out=out, in_=in_), reads, writes)

    def tt(self, eng, out, in0, in1, op, reads, writes):
        return self.P.op(eng, lambda e: e.tensor_tensor(out=out, in0=in0, in1=in1, op=op), reads, writes)

    def ts(self, eng, out, in0, s1, s2, op0, op1, reads, writes):
        if s2 is None:
            return self.P.op(eng, lambda e: e.tensor_scalar(out=out, in0=in0, scalar1=s1, scalar2=None, op0=op0), reads, writes)
        return self.P.op(eng, lambda e: e.tensor_scalar(out=out, in0=in0, scalar1=s1, scalar2=s2, op0=op0, op1=op1), reads, writes)

    def stt(self, eng, out, in0, scalar, in1, op0, op1, reads, writes):
        return self.P.op(eng, lambda e: e.scalar_tensor_tensor(out=out, in0=in0, scalar=scalar, in1=in1, op0=op0, op1=op1), reads, writes)

    def recip(self, out, in_, reads, writes):
        return self.P.op('dve', lambda e: e.reciprocal(out=out, in_=in_), reads, writes)

    def memset(self, eng, ap, val, writes):
        return self.P.op(eng, lambda e: e.memset(ap, val), (), writes)


class Ring:
    def __init__(self, tiles):
        self.tiles = tiles
        self.i = 0

    def next(self):
        t = self.tiles[self.i % len(self.tiles)]
        self.i += 1
        return t


class Tl:
    def __init__(self, t, key):
        self.t = t
        self.key = key

    def __getitem__(self, k):
        return self.t[k]


_UID = [0]


def _sb(kb, name, shape, dt):
    _UID[0] += 1
    return Tl(kb.es.enter_context(kb.nc.sbuf_tensor(f"s{_UID[0]}_{name}", shape, dt)), name)


def _ps(kb, name, shape, dt):
    _UID[0] += 1
    return Tl(kb.es.enter_context(kb.nc.psum_tensor(f"p{_UID[0]}_{name}", shape, dt)), name)


KB.sb = _sb
KB.ps = _ps


def make_ident(kb):
    identf = kb.sb("identf", [128, 128], F32)
    ident = kb.sb("ident", [128, 128], BF16)
    kb.memset('pool', identf[:], 1.0, ['identf'])
    kb.P.op('pool', lambda e: e.affine_select(out=identf[:], in_=identf[:], pattern=[[-1, 128]],
                                              compare_op=ALU.is_equal, fill=0.0, base=0, channel_multiplier=1),
            ['identf'], ['identf'])
    kb.copy('dve', ident[:], identf[:], ['identf'], ['ident'])
    return ident, identf


def phase_mods(kb, L, mods_dram):
    nc = kb.nc
    cv = kb.sb("m_cv", [128, 16], F32)
    sv = kb.sb("m_sv", [128, 16], F32)
    kb.dma('sp', cv[:], L['cvec'], [], ['m_cv'])
    kb.act(sv[:], cv[:], AF.Silu, ['m_cv'], ['m_sv'])
    lt = kb.sb("m_lt", [128, 8, 2], F32)
    for kc in range(8):
        kb.copy('dve', lt[:, kc, 0:1], sv[:, kc:kc + 1], ['m_sv'], [('m_lt', kc)])
        kb.copy('dve', lt[:, kc, 1:2], sv[:, 8 + kc:9 + kc], ['m_sv'], [('m_lt', kc)])
    brow = kb.sb("m_brow", [2, 6144], F32)
    kb.dma('sp', brow[0:1, :], L['ada_b'], [], [('m_brow', 0)])
    kb.dma('sp', brow[1:2, :], L['ada_b'], [], [('m_brow', 1)])
    res = kb.sb("m_res", [2, 6144], F32)
    wr = [kb.sb(f"m_w{i}", [128, 8, 512], F32) for i in range(2)]
    pm = [kb.ps(f"m_ps{i}", [128, 512], F32) for i in range(2)]
    for n in range(12):
        w = wr[n % 2]
        p = pm[n % 2]
        kb.dma('sp' if n % 2 == 0 else 'pool', w[:], L['ada_w'][:, n * 512:(n + 1) * 512].rearrange("(k p) n -> p k n", p=128),
               [], [w.key])
        for kc in range(8):
            kb.mm(p[0:2, :], lt[:, kc, :], w[:, kc, :], kc == 0, kc == 7, ['m_lt', w.key], [p.key])
        kb.tt('dve', res[:, n * 512:(n + 1) * 512], p[0:2, :], brow[:, n * 512:(n + 1) * 512], ALU.add,
              [p.key, 'm_brow'], [('m_res', n)])
    kb.dma('sp', mods_dram, res[:], ['m_res'], ['mods_dram'])


def load_rows(kb, mods_dram, L, g1key, g2key):
    out = {}
    tmp = kb.sb("r_tmp", [128, 1024], F32)
    gk = {1: kb.sb("r_g1", [128, 1024], F32), 2: kb.sb("r_g2", [128, 1024], F32)}
    kb.dma('sp', gk[1][:], L[g1key].partition_broadcast(128), [], ['r_g1'])
    kb.dma('sp', gk[2][:], L[g2key].partition_broadcast(128), [], ['r_g2'])
    for si, s in enumerate('lc'):
        for j, nm in enumerate(['S1', 'G1', 'A1', 'S2', 'G2', 'A2']):
            t = kb.sb(f"r_{s}{nm}", [128, 1024], F32)
            src = mods_dram[si:si + 1, j * 1024:(j + 1) * 1024].partition_broadcast(128)
            if nm[0] == 'G':
                kb.dma('sp', tmp[:], src, ['mods_dram'], ['r_tmp'])
                g = gk[int(nm[1])]
                kb.stt('dve', t[:], tmp[:], 1.0, g[:], ALU.add, ALU.mult, ['r_tmp', g.key], [t.key])
            else:
                kb.dma('sp', t[:], src, ['mods_dram'], [t.key])
            out[s + nm] = t
    return out


class Phase:
    def __init__(self, nc, name):
        self.nc = nc
        self.name = name

    def __enter__(self):
        self.kb = KB(self.nc)
        return self.kb

    def __exit__(self, et, ev, tb):
        if et is None:
            info = self.kb.P.emit()
            print('phase', self.name, info, flush=True)
        self.kb.es.close()
        if et is None:
            self.nc.all_engine_barrier()
        return False


def rings(kb, name, n, shape, dt, psum=False):
    mk = kb.ps if psum else kb.sb
    return Ring([mk(f"{name}{i}", shape, dt) for i in range(n)])


def norm_tile(kb, C, xt, rowsG, rowsS, width, rr, out_bf, src_is_psum_parts=None, gain_only=False):
    junk = rr['junk']
    ss = rr['ss'].next()
    rs = rr['rs'].next()
    h32 = rr['h32'].next()
    xa, xkeys = xt
    kb.act(junk[:, 0:width], xa, AF.Square, xkeys, [junk.key, ss.key], accum=ss[:])
    kb.ts('dve', rs[:], ss[:], 1.0 / width, EPS, ALU.mult, ALU.add, [ss.key], [rs.key])
    kb.act(rs[:], rs[:], AF.Sqrt, [rs.key], [rs.key])
    kb.recip(rs[:], rs[:], [rs.key], [rs.key])
    Ga, Gk = rowsG
    if rowsS is None:
        kb.stt('dve', out_bf, xa, rs[:], Ga, ALU.mult, ALU.mult, xkeys + [rs.key] + Gk, [C['_outkey']])
    else:
        Sa, Sk = rowsS
        kb.stt('dve', h32[:, 0:width], xa, rs[:], Ga, ALU.mult, ALU.mult, xkeys + [rs.key] + Gk, [h32.key])
        kb.tt('pool', out_bf, h32[:, 0:width], Sa, ALU.add, [h32.key] + Sk, [C['_outkey']])


def transpose_to(kb, C, src, srckey, nch, dst3, dstkey, eng='act'):
    psT = C['psT'].next()
    for kc in range(nch):
        kb.tr(psT[:, kc * 128:(kc + 1) * 128], src[:, kc * 128:(kc + 1) * 128], C['ident'][:],
              [srckey, 'ident'], [(psT.key, kc)])
    kb.copy(eng, dst3, psT[:, 0:nch * 128].rearrange("p (k t) -> p k t", k=nch), [psT.key], [dstkey])


def proj_fm(kb, ps, M, w, col0, nk, rhsT, bw, rkeys):
    for kc in range(nk):
        kb.mm(ps[0:M, 0:bw], w[:, kc, col0:col0 + M], rhsT[:, kc, 0:bw], kc == 0, kc == nk - 1,
              rkeys, [ps.key])


def rope_out(kb, C, psA, psB, tab, p0, p1, bw, outs):
    t1 = C['t1'].next()
    t2 = C['t2'].next()
    kb.tt('dve', t1[p0:p1, 0:bw], psA[p0:p1, 0:bw], tab[p0:p1, 0, 0:bw], ALU.mult, [psA.key, tab.key], [t1.key])
    kb.tt('dve', t2[p0:p1, 0:bw], psB[p0:p1, 0:bw], tab[p0:p1, 1, 0:bw], ALU.mult, [psB.key, tab.key], [t2.key])
    for i, (oa, ok) in enumerate(outs):
        kb.tt('pool' if i % 2 == 0 else 'dve', oa, t1[p0:p1, 0:bw], t2[p0:p1, 0:bw], ALU.add, [t1.key, t2.key], [ok])


def phase_E1(kb, L, S, mods_dram):
    C = {}
    C['ident'], _ = make_ident(kb)
    g1 = kb.sb("r_g1", [128, 1024], F32)
    kb.dma('sp', g1[:], L['n1g'].partition_broadcast(128), [], ['r_g1'])
    rows = {}
    for si, s in enumerate('lc'):
        tS = kb.sb(f"r_{s}S1", [128, 1024], F32)
        tG = kb.sb(f"r_{s}G1", [128, 1024], F32)
        kb.dma('sp', tS[:], mods_dram[si:si + 1, 0:1024].partition_broadcast(128), ['mods_dram'], [tS.key])
        kb.dma('sp', tG[:], mods_dram[si:si + 1, 1024:2048].partition_broadcast(128), ['mods_dram'], [tG.key])
        kb.stt('dve', tG[:], tG[:], 1.0, g1[:], ALU.add, ALU.mult, [tG.key, 'r_g1'], [tG.key])
        rows[s + 'S1'] = tS
        rows[s + 'G1'] = tG
    qng = kb.sb("r_qng", [128, 768], F32)
    kvng = kb.sb("r_kvng", [128, 256], F32)
    kb.dma('sp', qng[:], L['qng'].partition_broadcast(128), [], ['r_qng'])
    kb.dma('sp', kvng[:], L['kvng'].partition_broadcast(128), [], ['r_kvng'])
    win = kb.sb("w_in", [128, 8, W_IN_EXT], BF16)
    wuq = kb.sb("w_uq", [128, 6, 1536], BF16)
    wukv = kb.sb("w_ukv", [128, 2, 1024], BF16)
    for kc in range(8):
        kb.dma('pool', win[:, kc, :], L['w_in_ext'][kc * 128:(kc + 1) * 128, :], [], [('w_in', kc)])
    for kc in range(6):
        kb.dma('pool', wuq[:, kc, :], L['w_uq_ext'][kc * 128:(kc + 1) * 128, :], [], [('w_uq', kc)])
    for kc in range(2):
        kb.dma('pool', wukv[:, kc, :], L['w_ukv'][kc * 128:(kc + 1) * 128, :], [], [('w_ukv', kc)])
    wukv_v = wukv[:].rearrange("p k (h t d) -> p k h t d", h=8, t=2)
    C['psT'] = rings(kb, "psT", 1, [128, 1024], BF16, psum=True)
    psA = rings(kb, "psA", 2, [128, 512], F32, psum=True)
    psB = rings(kb, "psB", 2, [128, 512], F32, psum=True)
    psG = rings(kb, "psG", 2, [128, 512], F32, psum=True)
    C['t1'] = rings(kb, "t1_", 2, [128, 512], F32)
    C['t2'] = rings(kb, "t2_", 2, [128, 512], F32)
    rr = dict(junk=kb.sb("junk", [128, 1024], BF16), ss=rings(kb, "ss", 4, [128, 1], F32),
              rs=rings(kb, "rs", 4, [128, 1], F32), h32=rings(kb, "h32_", 2, [128, 1024], F32))
    xr = rings(kb, "xt", 2, [128, 1024], F32)
    hbr = rings(kb, "hb", 2, [128, 1024], BF16)
    hTr = rings(kb, "hT", 2, [128, 8, 512], BF16)
    cqnr = rings(kb, "cqn", 2, [128, 768], BF16)
    cqnT = kb.sb("cqnT", [128, 6, 512], BF16)
    ckvnr = rings(kb, "ckvn", 2, [128, 256], BF16)
    ckvnT = kb.sb("ckvnT", [128, 2, 512], BF16)
    st_qa = kb.sb("st_qa", [64, 8, 512], BF16)
    st_ka = kb.sb("st_ka", [64, 2, 512], BF16)
    st_va = kb.sb("st_va", [128, 4, 128], BF16)
    st_qm = kb.sb("st_qm", [96, 8, 512], BF16)
    st_km = kb.sb("st_km", [96, 8, 512], BF16)
    st_vm = kb.sb("st_vm", [128, 4, 512], BF16)
    tab64 = kb.sb("tab64", [64, 2, 512], F32)
    tab96 = kb.sb("tab96", [96, 2, 512], F32)

    def block(setname, blk, bw, do_q, kwin):
        s = 'c' if setname == 'ctx' else 'l'
        xsrc = L['x_' + setname]
        tcol = {'own': 0, 'oth': NQ, 'ctx': NQ + NO}[setname] + blk * 512
        nt = bw // 128
        hT = hTr.next()
        kb.dma('sp', tab96[:, :, 0:bw], L['tab96'][:, :, tcol:tcol + bw], [], ['tab96'])
        if do_q or kwin:
            kb.dma('sp', tab64[:, :, 0:bw], L['tab64'][:, :, tcol:tcol + bw], [], ['tab64'])
        for j in range(nt):
            xt = xr.next()
            r0 = blk * 512 + j * 128
            kb.dma('sp', xt[:], xsrc[r0:r0 + 128, :], ['x_' + setname], [xt.key])
            hb = hbr.next()
            C['_outkey'] = hb.key
            norm_tile(kb, C, (xt[:], [xt.key]), (rows[s + 'G1'][:], [rows[s + 'G1'].key]),
                      (rows[s + 'S1'][:], [rows[s + 'S1'].key]), 1024, rr, hb[:])
            transpose_to(kb, C, hb, hb.key, 8, hT[:, :, j * 128:(j + 1) * 128], (hT.key, j))
        hk = [hT.key, 'w_in']
        if do_q:
            for h in range(8):
                a = psA.next(); b = psB.next()
                proj_fm(kb, a, 64, win, h * 64, 8, hT, bw, hk)
                proj_fm(kb, b, 64, win, 1824 + h * 64, 8, hT, bw, hk)
                rope_out(kb, C, a, b, tab64, 0, 64, bw, [(st_qa[:, h, 0:bw], ('st_qa', h))])
            kb.dma('sp', S['qaT'][:, :, (NQ if setname == 'ctx' else blk * 512):(NQ if setname == 'ctx' else blk * 512) + bw].rearrange("h d t -> d h t"),
                   st_qa[:, :, 0:bw], ['st_qa'], [('qaT', setname, blk)])
        if kwin:
            for kv in range(2):
                a = psA.next(); b = psB.next()
                proj_fm(kb, a, 64, win, 512 + kv * 64, 8, hT, bw, hk)
                proj_fm(kb, b, 64, win, 2336 + kv * 64, 8, hT, bw, hk)
                rope_out(kb, C, a, b, tab64, 0, 64, bw, [(st_ka[:, kv, 0:bw], ('st_ka', kv))])
            for j in range(nt):
                g = psG.next()
                for kc in range(8):
                    kb.mm(g[:, 0:128], hT[:, kc, j * 128:(j + 1) * 128], win[:, kc, 640:768], kc == 0, kc == 7, hk, [g.key])
                kb.copy('act', st_va[:, j, :], g[:, 0:128], [g.key], [('st_va', j)])
            for (sc, ncol, dc) in kwin:
                kb.dma('sp', S['kaT'][:, :, dc:dc + ncol].rearrange("h d t -> d h t"), st_ka[:, :, sc:sc + ncol],
                       ['st_ka'], [('kaT', dc)])
                kb.dma('sp', S['va'][dc:dc + ncol, :].rearrange("(j p) c -> p j c", p=128),
                       st_va[:, sc // 128:(sc + ncol) // 128, :], ['st_va'], [('va', dc)])
        if do_q:
            for j in range(nt):
                a = psA.next(); b = psB.next()
                for hf, p in enumerate((a, b)):
                    for kc in range(8):
                        kb.mm(p[:, 0:384], hT[:, kc, j * 128:(j + 1) * 128], win[:, kc, 768 + hf * 384:768 + (hf + 1) * 384],
                              kc == 0, kc == 7, hk, [p.key])
                cqn = cqnr.next()
                ss1 = rr['ss'].next(); ss2 = rr['ss'].next(); rs = rr['rs'].next()
                kb.act(rr['junk'][:, 0:384], a[:, 0:384], AF.Square, [a.key], ['junk', ss1.key], accum=ss1[:])
                kb.act(rr['junk'][:, 0:384], b[:, 0:384], AF.Square, [b.key], ['junk', ss2.key], accum=ss2[:])
                kb.tt('dve', rs[:], ss1[:], ss2[:], ALU.add, [ss1.key, ss2.key], [rs.key])
                kb.ts('dve', rs[:], rs[:], 1.0 / 768, EPS, ALU.mult, ALU.add, [rs.key], [rs.key])
                kb.act(rs[:], rs[:], AF.Sqrt, [rs.key], [rs.key])
                kb.recip(rs[:], rs[:], [rs.key], [rs.key])
                kb.stt('dve', cqn[:, 0:384], a[:, 0:384], rs[:], qng[:, 0:384], ALU.mult, ALU.mult, [a.key, rs.key, 'r_qng'], [(cqn.key, 0)])
                kb.stt('dve', cqn[:, 384:768], b[:, 0:384], rs[:], qng[:, 384:768], ALU.mult, ALU.mult, [b.key, rs.key, 'r_qng'], [(cqn.key, 1)])
                transpose_to(kb, C, cqn, cqn.key, 6, cqnT[:, :, j * 128:(j + 1) * 128], ('cqnT', j))
            for h in range(8):
                a = psA.next(); b = psB.next()
                proj_fm(kb, a, 96, wuq, h * 96, 6, cqnT, bw, ['cqnT', 'w_uq'])
                proj_fm(kb, b, 96, wuq, 768 + h * 96, 6, cqnT, bw, ['cqnT', 'w_uq'])
                rope_out(kb, C, a, b, tab96, 0, 96, bw, [(st_qm[:, h, 0:bw], ('st_qm', h))])
            qc = NQ if setname == 'ctx' else blk * 512
            kb.dma('sp', S['qmT'][:, :, qc:qc + bw].rearrange("h d t -> d h t"), st_qm[:, :, 0:bw], ['st_qm'], [('qmT', setname, blk)])
        for j in range(nt):
            g = psG.next()
            for kc in range(8):
                kb.mm(g[:, 0:256], hT[:, kc, j * 128:(j + 1) * 128], win[:, kc, 1536:1792], kc == 0, kc == 7, hk, [g.key])
            ckvn = ckvnr.next()
            C['_outkey'] = ckvn.key
            norm_tile(kb, C, (g[:, 0:256], [g.key]), (kvng[:], ['r_kvng']), None, 256, rr, ckvn[:])
            transpose_to(kb, C, ckvn, ckvn.key, 2, ckvnT[:, :, j * 128:(j + 1) * 128], ('ckvnT', j), eng='dve')
        for h in range(8):
            g = psG.next()
            proj_fm(kb, g, 64, wukv, h * 128, 2, ckvnT, bw, ['ckvnT', 'w_ukv'])
            kb.copy('act' if h % 2 == 0 else 'dve', st_km[0:64, h, 0:bw], g[0:64, 0:bw], [g.key], [('st_km', h)])
        for j in range(nt):
            g = psG.next()
            for kc in range(2):
                kb.mm(g[:, 0:512], ckvnT[:, kc, j * 128:(j + 1) * 128], wukv_v[:, kc, :, 1, :], kc == 0, kc == 1,
                      ['ckvnT', 'w_ukv'], [g.key])
            kb.copy('act', st_vm[:, j, :], g[:, 0:512], [g.key], [('st_vm', j)])
        a = psA.next(); b = psB.next()
        proj_fm(kb, a, 96, win, 1728, 8, hT, bw, hk)
        proj_fm(kb, b, 96, win, 2400, 8, hT, bw, hk)
        rope_out(kb, C, a, b, tab96, 64, 96, bw, [(st_km[64:96, h, 0:bw], ('st_km', 8 + h)) for h in range(8)])
        kc0 = tcol
        kb.dma('sp', S['kmT'][:, :, kc0:kc0 + bw].rearrange("h d t -> d h t"), st_km[:, :, 0:bw], ['st_km'], [('kmT', setname, blk)])
        kb.dma('sp', S['vm'][kc0:kc0 + bw, :].rearrange("(j p) c -> p j c", p=128), st_vm[:, 0:nt, :], ['st_vm'], [('vm', setname, blk)])

    for blk in range(NQ // 512):
        block('own', blk, 512, True, [(0, 512, 128 + blk * 512)])
    for blk in range(NO // 512):
        kw = []
        if blk == 0:
            kw = [(0, 128, 128 + NQ)]
        if blk == NO // 512 - 1:
            kw = [(384, 128, 0)]
        block('oth', blk, 512, False, kw)
    block('ctx', 0, 256, True, [(0, 256, 128 + NQ + 128)])


def make_sel(kb):
    sel = kb.sb("sel", [128, 64], F32)
    kb.memset('pool', sel[:], 0.0, ['sel'])
    kb.memset('pool', sel[64:128, :], 1.0 / 64, ['sel'])
    return sel


def attn_finalize(kb, C, ops, N, esink, out_ap, out_key, split=None):
    osb = C['osb'].next()
    den = C['psD'].next()
    rec = C['rec'].next()
    kb.copy('dve', osb[:, 0:N], ops[:, 0:N], [ops.key], [osb.key])
    kb.mm(den[0:64, 0:N], C['sel'][:, :], osb[:, 0:N], True, True, ['sel', osb.key], [den.key])
    if esink is not None:
        ea, ek = esink
        kb.tt('dve', rec[0:64, 0:N], den[0:64, 0:N], ea, ALU.add, [den.key, ek], [rec.key])
        kb.recip(rec[0:64, 0:N], rec[0:64, 0:N], [rec.key], [rec.key])
    else:
        kb.recip(rec[0:64, 0:N], den[0:64, 0:N], [den.key], [rec.key])
    i0 = osb[0:64, 0:N]
    i1 = rec[0:64, 0:N]
    if split:
        i0 = i0.rearrange("p (h t) -> p h t", h=split)
        i1 = i1.rearrange("p (h t) -> p h t", h=split)
    kb.tt('pool', out_ap, i0, i1, ALU.mult, [osb.key, rec.key], [out_key])


def attn_unit(kb, C, qrhs, qkeys, N, tiles, scale, esink, out_ap, out_key, split=None):
    LA = 2
    ops = C['psO'].next()
    nt = len(tiles)
    pts = [None] * nt
    for i in range(nt + LA):
        if i < nt:
            ka, kk, va, vk, mask = tiles[i]
            pss = C['psS'].next()
            pt = C['pT'].next()
            pts[i] = pt
            kb.mm(pss[:, 0:N], ka, qrhs, True, True, kk + qkeys, [pss.key])
            kb.act(pt[:, 0:N], pss[:, 0:N], AF.Exp, [pss.key], [pt.key], scale=scale)
            if mask is not None:
                ma, mk_ = mask
                kb.tt('pool', pt[:, 0:N], pt[:, 0:N], ma, ALU.mult, [pt.key] + mk_, [pt.key])
        if i == min(LA, nt) - 1 and C.get('pending') is not None:
            C['pending']()
            C['pending'] = None
        j = i - LA
        if 0 <= j < nt:
            ka, kk, va, vk, mask = tiles[j]
            kb.mm(ops[:, 0:N], va, pts[j][:, 0:N], j == 0, j == nt - 1, vk + [pts[j].key], [ops.key])
    C['pending'] = lambda: attn_finalize(kb, C, ops, N, esink, out_ap, out_key, split)


def attn_flush(kb, C):
    if C.get('pending') is not None:
        C['pending']()
        C['pending'] = None


def attn_common(kb, C):
    C['sel'] = make_sel(kb)
    C['psS'] = rings(kb, "psS", 3, [128, 512], F32, psum=True)
    C['psO'] = rings(kb, "psO", 2, [128, 512], F32, psum=True)
    C['psD'] = rings(kb, "psD", 1, [128, 512], F32, psum=True)
    C['pT'] = rings(kb, "pT", 5, [128, 512], BF16)
    C['osb'] = rings(kb, "osb", 2, [128, 512], F32)
    C['rec'] = rings(kb, "rec", 2, [64, 512], F32)


def phase_E2a(kb, L, S):
    C = {}
    attn_common(kb, C)
    kaT = kb.sb("kaT_sb", [64, 2, NKA], BF16)
    va1 = kb.sb("va1_sb", [128, NKA // 128, 2, 128], BF16)
    kb.memset('pool', va1[:], 1.0, ['va1_sb'])
    kb.dma('sp', kaT[:], S['kaT'].rearrange("h d t -> d h t"), ['kaT'], ['kaT_sb'])
    for g in range(2):
        kb.dma('sp', va1[:, :, g, 0:64], S['va'][:, g * 64:(g + 1) * 64].rearrange("(t p) d -> p t d", p=128), ['va'], [('va1_sb', g)])
    msk = kb.sb("wmask", [128, 4, 512], BF16)
    kb.dma('pool', msk[:], L['wmask'], [], ['wmask'])
    esink = kb.sb("esink", [64, 2, 512], F32)
    kb.dma('sp', esink[:], L['sinkb'], [], ['esink'])
    kb.act(esink[:], esink[:], AF.Exp, ['esink'], ['esink'])
    qr = rings(kb, "qa_blk", 2, [64, 8, 512], BF16)
    so = rings(kb, "so_a", 2, [64, 8, 512], BF16)
    scale = 64 ** -0.5
    nkt = NKA // 128
    ctx_t = [(128 + NQ + 128) // 128, (128 + NQ + 128) // 128 + 1]

    def tile(g, t, mask):
        return (kaT[:, g, t * 128:(t + 1) * 128], ['kaT_sb'], va1[:, t, g, :], ['va1_sb'], mask)

    for qb in range(NQ // 512 + 1):
        is_ctx = qb == NQ // 512
        bw = 256 if is_ctx else 512
        q = qr.next()
        o = so.next()
        kb.dma('sp', q[:, :, 0:bw], S['qaT'][:, :, qb * 512:qb * 512 + bw].rearrange("h d t -> d h t"), ['qaT'], [q.key])
        for j in range(bw // 128):
            n = qb * 4 + j
            for g in range(2):
                qrhs = q[:, 4 * g:4 * g + 4, j * 128:(j + 1) * 128]
                if is_ctx:
                    tl = [tile(g, ctx_t[0], None), tile(g, ctx_t[1], None)]
                else:
                    mp = msk[:, 2 if n == 0 else 0, :]
                    mn = msk[:, 3 if n == NQ // 128 - 1 else 1, :]
                    tl = [tile(g, n, (mp, ['wmask'])), tile(g, n + 1, None), tile(g, n + 2, (mn, ['wmask'])),
                          tile(g, ctx_t[0], None), tile(g, ctx_t[1], None)]
                attn_unit(kb, C, qrhs, [q.key], 512, tl, scale, (esink[:, g, :], 'esink'),
                          o[:, 4 * g:4 * g + 4, j * 128:(j + 1) * 128], (o.key, j, g), split=4)
        attn_flush(kb, C)
        kb.dma('sp', S['oT'][0:8, :, qb * 512:qb * 512 + bw].rearrange("h d t -> d h t"), o[:, :, 0:bw], [o.key], [('oT', 'a', qb)])


def phase_E2b(kb, L, S):
    C = {}
    attn_common(kb, C)
    kr = rings(kb, "km_sb", 2, [96, NKM], BF16)
    vr = rings(kb, "vm1_sb", 2, [128, NKM // 128, 128], BF16)
    qr = rings(kb, "qm_sb", 2, [96, NQA], BF16)
    for v in vr.tiles:
        kb.memset('pool', v[:], 1.0, [v.key])
    so = rings(kb, "so_b", 2, [64, 512], BF16)
    scale = 96 ** -0.5
    nkt = NKM // 128
    for h in range(8):
        k = kr.next(); v = vr.next(); q = qr.next()
        kb.dma('sp', k[:], S['kmT'][h], ['kmT'], [k.key])
        kb.dma('sp', q[:], S['qmT'][h], ['qmT'], [q.key])
        kb.dma('pool', v[:, :, 0:64], S['vm'][:, h * 64:(h + 1) * 64].rearrange("(t p) d -> p t d", p=128), ['vm', v.key], [v.key])
        for qb in range(NQ // 512 + 1):
            is_ctx = qb == NQ // 512
            bw = 256 if is_ctx else 512
            trange = range(nkt - 2, nkt) if is_ctx else range(nkt)
            tl = [(k[:, t * 128:(t + 1) * 128], [k.key], v[:, t, :], [v.key], None) for t in trange]
            o = so.next()
            attn_unit(kb, C, q[:, qb * 512:qb * 512 + bw], [q.key], bw, tl, scale, None, o[:, 0:bw], o.key)
            if C.get('pend_dma') is not None:
                pass
            C.setdefault('dmaq', []).append((S['oT'][8 + h, :, qb * 512:qb * 512 + bw], o, bw, ('oT', 'b', h, qb)))
            if len(C['dmaq']) > 1:
                da, do, dbw, dk = C['dmaq'].pop(0)
                kb.dma('sp', da, do[:, 0:dbw], [do.key], [dk])
    attn_flush(kb, C)
    for (da, do, dbw, dk) in C['dmaq']:
        kb.dma('sp', da, do[:, 0:dbw], [do.key], [dk])


DBG_OUT = None
NTOK = NQ + NCX
NTT = NTOK // 128
MB = 256
NBLK = (NTOK * 4 + 32 * (MB - 1)) // MB
NSLOT = NBLK * MB
MAXB = NTOK * 4 // MB + 1


def moe_scratch(nc):
    def dr(name, shape, dt):
        return nc.dram_tensor(name, shape, dt, kind="Internal").ap()
    return dict(x_mid=dr("x_mid", [NTOK, 1024], F32), h2b=dr("h2b", [NTOK, 1024], BF16),
                rt_slot=dr("rt_slot", [128, NTT * 4], I32), rt_gk=dr("rt_gk", [128, NTT * 4], F32),
                rt_woff=dr("rt_woff", [128, NBLK * 8], I32), rt_boff=dr("rt_boff", [128, NBLK], I32),
                rt_eoff=dr("rt_eoff", [128, NBLK], I32),
                xs=dr("xs", [NSLOT, 1024], BF16), ys=dr("ys", [NSLOT, 1024], F32))


def phase_OP(kb, L, S, M, mods_dram):
    C = {}
    C['ident'], identf = make_ident(kb)
    g2 = kb.sb("r_g2", [128, 1024], F32)
    kb.dma('sp', g2[:], L['n2g'].partition_broadcast(128), [], ['r_g2'])
    rows = {}
    for si, s in enumerate('lc'):
        for nm, j in (('A1', 2), ('S2', 3), ('G2', 4)):
            t = kb.sb(f"r_{s}{nm}", [128, 1024], F32)
            kb.dma('sp', t[:], mods_dram[si:si + 1, j * 1024:(j + 1) * 1024].partition_broadcast(128), ['mods_dram'], [t.key])
            if nm == 'G2':
                kb.stt('dve', t[:], t[:], 1.0, g2[:], ALU.add, ALU.mult, [t.key, 'r_g2'], [t.key])
            rows[s + nm] = t
    wout = kb.sb("w_out_sb", [64, 16, 1024], BF16)
    for h in range(16):
        kb.dma('pool', wout[:, h, :], L['w_out'][h * 64:(h + 1) * 64, :], [], [('w_out_sb', h)])
    rw = kb.sb("rw", [128, 8, 32], F32)
    kb.dma('sp', rw[:], L['router_w'].rearrange("(k p) e -> p k e", p=128), [], ['rw'])
    rb = kb.sb("rb", [128, 32], F32)
    kb.dma('sp', rb[:], L['router_b'].partition_broadcast(128), [], ['rb'])
    ltf = kb.sb("ltf", [128, 128], F32)
    lt = kb.sb("lt", [128, 128], BF16)
    ones = kb.sb("ones", [128, 128], BF16)
    kb.memset('pool', ltf[:], 1.0, ['ltf'])
    kb.P.op('pool', lambda e: e.affine_select(out=ltf[:], in_=ltf[:], pattern=[[1, 128]], compare_op=ALU.is_gt, fill=0.0,
                                              base=0, channel_multiplier=-1), ['ltf'], ['ltf'])
    kb.copy('dve', lt[:], ltf[:], ['ltf'], ['lt'])
    kb.memset('pool', ones[:], 1.0, ['ones'])
    zt = kb.sb("zt", [128, 8192], BF16)
    kb.memset('pool', zt[:], 0.0, ['zt'])
    xs_v = M['xs'][0:(NSLOT // 1024) * 1024, :].rearrange("(a p r) c -> a p (r c)", p=128, r=8)
    for a in range(NSLOT // 1024):
        kb.dma('sp', xs_v[a], zt[:], ['zt'], [('xs', 'z', a)])
    rem = NSLOT - (NSLOT // 1024) * 1024
    if rem:
        kb.dma('sp', M['xs'][NSLOT - rem:NSLOT, :].rearrange("(p r) c -> p (r c)", p=128), zt[:, 0:rem // 128 * 1024], ['zt'], [('xs', 'z', 'r')])
    if os.environ.get('OP_STOP') == '1':
        return
    lg = kb.sb("lg", [128, NTT, 32], F32)
    mx8 = kb.sb("mx8", [128, NTT, 8], F32)
    gat = kb.sb("gat", [128, NTT, 32], F32)
    pos = kb.sb("pos", [128, NTT, 32], F32)
    base = rings(kb, "base", 2, [128, 32], F32)
    b0 = base.next()
    kb.memset('dve', b0[:], 0.0, [b0.key])
    psM = rings(kb, "psM", 2, [128, 512], F32, psum=True)
    psR = rings(kb, "psR", 2, [128, 512], F32, psum=True)
    psL = rings(kb, "psL", 2, [128, 64], F32, psum=True)
    xr = rings(kb, "xt", 2, [128, 1024], F32)
    tr_ = rings(kb, "tmp", 2, [128, 1024], F32)
    xmr = rings(kb, "xm", 2, [128, 1024], F32)
    h2r = rings(kb, "h2f", 2, [128, 1024], F32)
    h2br = rings(kb, "h2bt", 2, [128, 1024], BF16)
    h2Tr = rings(kb, "h2T", 2, [128, 8, 128], F32)
    obr = rings(kb, "o_blk", 2, [64, 16, 512], BF16)
    rr = dict(junk=kb.sb("junk", [128, 1024], BF16), ss=rings(kb, "ss", 4, [128, 1], F32),
              rs=rings(kb, "rs", 4, [128, 1], F32))
    sm = rings(kb, "sm", 4, [128, 1], F32)
    ex = rings(kb, "ex", 2, [128, 32], F32)
    mk = rings(kb, "mk", 2, [128, 32], F32)
    mkb = rings(kb, "mkb", 2, [128, 32], BF16)
    ob = None
    for t in range(NTT):
        s = 'l' if t < NT_OWN else 'c'
        if t % 4 == 0:
            bw = 512 if t < NT_OWN else 256
            ob = obr.next()
            kb.dma('sp', ob[:, :, 0:bw], S['oT'][:, :, t * 128:t * 128 + bw].rearrange("h d t -> d h t"), ['oT'], [ob.key])
        j = t % 4
        xt = xr.next()
        xsrc = L['x_own'][t * 128:(t + 1) * 128, :] if t < NT_OWN else L['x_ctx'][(t - NT_OWN) * 128:(t - NT_OWN + 1) * 128, :]
        kb.dma('sp', xt[:], xsrc, [], [xt.key])
        pa = psM.next(); pb = psM.next()
        for nh, p in enumerate((pa, pb)):
            for h in range(16):
                kb.mm(p[:, :], ob[:, h, j * 128:(j + 1) * 128], wout[:, h, nh * 512:(nh + 1) * 512], h == 0, h == 15,
                      [ob.key, 'w_out_sb'], [p.key])
        tmp = tr_.next(); xm = xmr.next()
        A1 = rows[s + 'A1']
        kb.tt('dve', tmp[:, 0:512], pa[:, :], A1[:, 0:512], ALU.mult, [pa.key, A1.key], [(tmp.key, 0)])
        kb.tt('dve', tmp[:, 512:1024], pb[:, :], A1[:, 512:1024], ALU.mult, [pb.key, A1.key], [(tmp.key, 1)])
        kb.tt('pool', xm[:], xt[:], tmp[:], ALU.add, [xt.key, tmp.key], [xm.key])
        kb.dma('sp', M['x_mid'][t * 128:(t + 1) * 128, :], xm[:], [xm.key], [('x_mid', t)])
        if os.environ.get('OP_STOP') == '2':
            continue
        h2 = h2r.next()
        C['_outkey'] = h2.key
        ss = rr['ss'].next(); rs = rr['rs'].next()
        kb.act(rr['junk'][:], xm[:], AF.Square, [xm.key], ['junk', ss.key], accum=ss[:])
        kb.ts('dve', rs[:], ss[:], 1.0 / 1024, EPS, ALU.mult, ALU.add, [ss.key], [rs.key])
        kb.act(rs[:], rs[:], AF.Sqrt, [rs.key], [rs.key])
        kb.recip(rs[:], rs[:], [rs.key], [rs.key])
        G2 = rows[s + 'G2']; S2 = rows[s + 'S2']
        kb.stt('dve', h2[:], xm[:], rs[:], G2[:], ALU.mult, ALU.mult, [xm.key, rs.key, G2.key], [h2.key])
        kb.tt('pool', h2[:], h2[:], S2[:], ALU.add, [h2.key, S2.key], [h2.key])
        h2b = h2br.next()
        kb.copy('act', h2b[:], h2[:], [h2.key], [h2b.key])
        kb.dma('sp', M['h2b'][t * 128:(t + 1) * 128, :], h2b[:], [h2b.key], [('h2b', t)])
        ra = psR.next(); rbk = psR.next()
        for kc in range(8):
            p = ra if kc < 4 else rbk
            kb.tr(p[:, (kc % 4) * 128:(kc % 4 + 1) * 128], h2[:, kc * 128:(kc + 1) * 128], identf[:], [h2.key, 'identf'], [(p.key, kc % 4)])
        h2T = h2Tr.next()
        kb.copy('act', h2T[:, 0:4, :], ra[:, :].rearrange("p (k t) -> p k t", k=4), [ra.key], [(h2T.key, 0)])
        kb.copy('dve', h2T[:, 4:8, :], rbk[:, :].rearrange("p (k t) -> p k t", k=4), [rbk.key], [(h2T.key, 1)])
        pl = psL.next()
        for kc in range(8):
            kb.mm(pl[:, 0:32], h2T[:, kc, :], rw[:, kc, :], kc == 0, kc == 7, [h2T.key, 'rw'], [pl.key])
        kb.tt('dve', lg[:, t, :], pl[:, 0:32], rb[:], ALU.add, [pl.key, 'rb'], [('lg', t)])
        kb.P.op('dve', (lambda tt_: (lambda e: e.max(out=mx8[:, tt_, :], in_=lg[:, tt_, :])))(t), [('lg', t)], [('mx8', t)])
        m = mk.next(); mb_ = mkb.next(); e_ = ex.next(); s1 = sm.next(); s2 = sm.next()
        kb.ts('dve', m[:], lg[:, t, :], mx8[:, t, 3:4], None, ALU.is_ge, None, [('lg', t), ('mx8', t)], [m.key])
        kb.ts('dve', s1[:], mx8[:, t, 0:1], -1.0, None, ALU.mult, None, [('mx8', t)], [s1.key])
        kb.act(e_[:], lg[:, t, :], AF.Exp, [('lg', t), s1.key], [e_.key], bias=s1[:], scale=1.0)
        kb.tt('dve', e_[:], e_[:], m[:], ALU.mult, [e_.key, m.key], [e_.key])
        kb.P.op('dve', (lambda ea, sa: (lambda e: e.reduce_sum(out=sa[:], in_=ea[:], axis=AX.X)))(e_, s2), [e_.key], [s2.key])
        kb.recip(s2[:], s2[:], [s2.key], [s2.key])
        kb.ts('dve', gat[:, t, :], e_[:], s2[:], None, ALU.mult, None, [e_.key, s2.key], [('gat', t)])
        kb.copy('dve', mb_[:], m[:], [m.key], [mb_.key])
        pp = psL.next()
        kb.mm(pp[:, 0:32], lt[:], mb_[:], True, True, ['lt', mb_.key], [pp.key])
        kb.mm(pp[:, 32:64], ones[:], mb_[:], True, True, ['ones', mb_.key], [pp.key])
        bnew = base.next()
        kb.tt('dve', pos[:, t, :], pp[:, 0:32], b0[:], ALU.add, [pp.key, b0.key], [('pos', t)])
        kb.tt('dve', bnew[:], pp[:, 32:64], b0[:], ALU.add, [pp.key, b0.key], [bnew.key])
        b0 = bnew
    if os.environ.get('OP_STOP') in ('2', '3'):
        return
    cnt = b0
    thr = kb.sb("thr", [128, MAXB + NBLK], F32)
    kb.P.op('pool', lambda e: e.iota(thr[:, 0:MAXB], pattern=[[MB, MAXB]], base=0, channel_multiplier=0, allow_small_or_imprecise_dtypes=True), [], [('thr', 0)])
    kb.P.op('pool', lambda e: e.iota(thr[:, MAXB:MAXB + NBLK], pattern=[[MB, NBLK]], base=0, channel_multiplier=0, allow_small_or_imprecise_dtypes=True), [], [('thr', 1)])
    cmp_ = kb.sb("cmp", [128, MAXB + NBLK], F32)
    nb = kb.sb("nb", [128, 32], F32)
    for e in range(32):
        kb.ts('dve', cmp_[:, 0:MAXB], thr[:, 0:MAXB], cnt[:, e:e + 1], None, ALU.is_lt, None, [('thr', 0), cnt.key], [('cmp', 0)])
        kb.P.op('dve', (lambda ee: (lambda en: en.reduce_sum(out=nb[:, ee:ee + 1], in_=cmp_[:, 0:MAXB], axis=AX.X)))(e), [('cmp', 0)], [('nb', e)])
    pend = rings(kb, "pend", 2, [128, 32], F32)
    pe0 = pend.next()
    kb.ts('dve', pe0[:], nb[:], float(MB), None, ALU.mult, None, ['nb'], [pe0.key])
    padded = kb.sb("padded", [128, 32], F32)
    kb.copy('dve', padded[:], pe0[:], [pe0.key], ['padded'])
    sh = 1
    while sh < 32:
        pe1 = pend.next()
        kb.copy('dve', pe1[:], pe0[:], [pe0.key], [pe1.key])
        kb.tt('dve', pe1[:, sh:32], pe0[:, sh:32], pe0[:, 0:32 - sh], ALU.add, [pe0.key], [pe1.key])
        pe0 = pe1
        sh *= 2
    pstart = kb.sb("pstart", [128, 32], F32)
    kb.tt('dve', pstart[:], pe0[:], padded[:], ALU.subtract, [pe0.key, 'padded'], ['pstart'])
    be = kb.sb("be", [128, NBLK], F32)
    kb.memset('dve', be[:], 0.0, ['be'])
    for e in range(32):
        kb.ts('dve', cmp_[:, MAXB:MAXB + NBLK], thr[:, MAXB:MAXB + NBLK], pe0[:, e:e + 1], None, ALU.is_ge, None, [('thr', 1), pe0.key], [('cmp', 1)])
        kb.tt('dve', be[:], be[:], cmp_[:, MAXB:MAXB + NBLK], ALU.add, ['be', ('cmp', 1)], ['be'])
    kb.ts('dve', be[:], be[:], 31.0, None, ALU.min, None, ['be'], ['be'])
    pidx = kb.sb("pidx", [128, 1], F32)
    kb.P.op('pool', lambda e: e.iota(pidx[:], pattern=[[0, 1]], base=0, channel_multiplier=1, allow_small_or_imprecise_dtypes=True), [], ['pidx'])
    wof = kb.sb("wof", [128, NBLK, 8], F32)
    wofi = kb.sb("wofi", [128, NBLK, 8], I32)
    bof = kb.sb("bof", [128, NBLK], F32)
    bofi = kb.sb("bofi", [128, NBLK], I32)
    eofi = kb.sb("eofi", [128, NBLK], I32)
    kb.ts('dve', bof[:], be[:], 1024.0, pidx[:], ALU.mult, ALU.add, ['be', 'pidx'], ['bof'])
    for kc in range(8):
        kb.ts('dve', wof[:, :, kc], bof[:], float(kc * 128), None, ALU.add, None, ['bof'], [('wof', kc)])
    kb.copy('dve', wofi[:], wof[:], ['wof'], ['wofi'])
    kb.ts('dve', bof[:], be[:], 128.0, pidx[:], ALU.mult, ALU.add, ['be', 'pidx', 'wof'], ['bof'])
    kb.copy('dve', bofi[:], bof[:], ['bof'], ['bofi'])
    kb.copy('dve', eofi[:], be[:], ['be'], ['eofi'])
    kb.dma('sp', M['rt_woff'], wofi[:].rearrange("p b k -> p (b k)"), ['wofi'], ['rt_woff'])
    kb.dma('sp', M['rt_boff'], bofi[:], ['bofi'], ['rt_boff'])
    kb.dma('sp', M['rt_eoff'], eofi[:], ['eofi'], ['rt_eoff'])
    if os.environ.get('OP_STOP') == '4':
        return
    C['dbg'] = dict(cnt=cnt, be=be, pstart=pstart, lg=lg, mx8=mx8)
    slf = kb.sb("slf", [128, NTT, 4], F32)
    C['dbg']['slf'] = slf
    sli = kb.sb("sli", [128, NTT, 4], I32)
    gk = kb.sb("gk", [128, NTT, 4], F32)
    dst = rings(kb, "dst", 2, [128, 32], F32)
    oh = rings(kb, "oh", 2, [128, 32], F32)
    pr = rings(kb, "pr", 2, [128, 32], F32)
    for t in range(NTT):
        d_ = dst.next()
        kb.tt('dve', d_[:], pos[:, t, :], pstart[:], ALU.add, [('pos', t), 'pstart'], [d_.key])
        for k in range(4):
            o_ = oh.next(); p_ = pr.next()
            kb.ts('dve', o_[:], lg[:, t, :], mx8[:, t, k:k + 1], None, ALU.is_equal, None, [('lg', t), ('mx8', t)], [o_.key])
            kb.tt('dve', p_[:], o_[:], d_[:], ALU.mult, [o_.key, d_.key], [p_.key])
            kb.P.op('dve', (lambda pa_, tt_, kk: (lambda e: e.reduce_sum(out=slf[:, tt_, kk:kk + 1], in_=pa_[:], axis=AX.X)))(p_, t, k), [p_.key], [('slf', t, k)])
            p2 = pr.next()
            kb.tt('dve', p2[:], o_[:], gat[:, t, :], ALU.mult, [o_.key, ('gat', t)], [p2.key])
            kb.P.op('dve', (lambda pa_, tt_, kk: (lambda e: e.reduce_sum(out=gk[:, tt_, kk:kk + 1], in_=pa_[:], axis=AX.X)))(p2, t, k), [p2.key], [('gk', t, k)])
        kb.ts('dve', slf[:, t, :], slf[:, t, :], 0.0, float(NSLOT - 1), ALU.max, ALU.min, [('slf', t, 0), ('slf', t, 1), ('slf', t, 2), ('slf', t, 3)], [('slf', t, 'c')])
        kb.copy('dve', sli[:, t, :], slf[:, t, :], [('slf', t, 'c')], [('sli', t)])
        if os.environ.get('OP_NOSCATTER'):
            continue
        h2b = h2br.next()
        kb.dma('sp', h2b[:], M['h2b'][t * 128:(t + 1) * 128, :], ['h2b'], [h2b.key])
        for k in range(4):
            kb.P.op('pool', (lambda hb, tt_, kk: (lambda e: e.indirect_dma_start(
                out=M['xs'], out_offset=bass.IndirectOffsetOnAxis(ap=sli[:, tt_, kk:kk + 1], axis=0),
                in_=hb[:], in_offset=None)))(h2b, t, k),
                [h2b.key, ('sli', t), 'xs'], [('xs', 's', t, k)], dma=True)
    kb.dma('sp', M['rt_slot'], sli[:].rearrange("p t k -> p (t k)"), ['sli'], ['rt_slot'])
    kb.dma('sp', M['rt_gk'], gk[:].rearrange("p t k -> p (t k)"), ['gk'], ['rt_gk'])
    if DBG_OUT is not None:
        for nm, tl, n in (('cnt', cnt, 32), ('be', be, NBLK), ('pstart', pstart, 32), ('slf', slf, NTT * 4), ('lg', lg, NTT * 32), ('mx8', mx8, NTT * 8)):
            o = kb.nc.dram_tensor("dbg_" + nm, [128, n], F32, kind="ExternalOutput").ap()
            src = tl[:] if len(tl[:].shape) == 2 else tl[:].rearrange("p a b -> p (a b)")
            kb.dma('sp', o, src, [tl.key], [])


def phase_MX(kb, L, M):
    C = {}
    C['ident'], _ = make_ident(kb)
    wofi = kb.sb("wofi", [128, NBLK * 8], I32)
    bofi = kb.sb("bofi", [128, NBLK], I32)
    eofi = kb.sb("eofi", [128, NBLK], I32)
    kb.dma('sp', wofi[:], M['rt_woff'], ['rt_woff'], ['wofi'])
    kb.dma('sp', bofi[:], M['rt_boff'], ['rt_boff'], ['bofi'])
    kb.dma('sp', eofi[:], M['rt_eoff'], ['rt_eoff'], ['eofi'])
    w1r = rings(kb, "w1sb", 2, [128, 8, 2048], BF16)
    w2r = rings(kb, "w2sb", 2, [128, 8, 1024], BF16)
    b1r = rings(kb, "b1sb", 2, [128, 16], F32)
    b2r = rings(kb, "b2sb", 2, [128, 1024], F32)
    xbr = rings(kb, "xblk", 2, [128, 2, 1024], BF16)
    xTr = rings(kb, "xT", 2, [128, 8, MB], BF16)
    aTr = rings(kb, "aT", 2, [128, 8, MB], BF16)
    yr = rings(kb, "yblk", 2, [128, 1024], F32)
    C['psT'] = rings(kb, "psT", 2, [128, 1024], BF16, psum=True)
    psU = rings(kb, "psU", 4, [128, 512], F32, psum=True)
    psY = rings(kb, "psY", 2, [128, 512], F32, psum=True)
    gr = rings(kb, "g_", 3, [128, MB], F32)
    sr = rings(kb, "sg_", 3, [128, MB], F32)
    lr = rings(kb, "l_", 3, [128, MB], F32)

    def gather(out_ap, table, off_ap, rkeys, wkey):
        kb.P.op('pool', lambda e: e.indirect_dma_start(out=out_ap, out_offset=None, in_=table,
                                                       in_offset=bass.IndirectOffsetOnAxis(ap=off_ap, axis=0)),
                rkeys, [wkey], dma=True)

    def load_w(b):
        w1 = w1r.next(); w2 = w2r.next(); b1 = b1r.next(); b2 = b2r.next()
        for kc in range(8):
            gather(w1[:, kc, :], L['w1'], wofi[:, b * 8 + kc:b * 8 + kc + 1], ['wofi'], (w1.key, kc))
        for kc in range(8):
            gather(w2[:, kc, :], L['w2'], wofi[:, b * 8 + kc:b * 8 + kc + 1], ['wofi'], (w2.key, kc))
        gather(b1[:], L['b1'], bofi[:, b:b + 1], ['bofi'], b1.key)
        gather(b2[:], L['b2'], eofi[:, b:b + 1], ['eofi'], b2.key)
        return w1, w2, b1, b2

    NB_ = int(os.environ.get('MX_NB', NBLK))
    nxt = load_w(0)
    for b in range(NB_):
        w1, w2, b1, b2 = nxt
        xb = xbr.next()
        kb.dma('sp', xb[:], M['xs'][b * MB:(b + 1) * MB, :].rearrange("(j p) c -> p j c", p=128), ['xs'], [xb.key])
        if b + 1 < NB_:
            nxt = load_w(b + 1)
        xT = xTr.next()
        for j in range(MB // 128):
            transpose_to(kb, C, xb[:, j, :], xb.key, 8, xT[:, :, j * 128:(j + 1) * 128], (xT.key, j), eng='act' if j == 0 else 'dve')
        aT = aTr.next()
        for c in range(8):
            pg = psU.next(); pl = psU.next()
            for kc in range(8):
                kb.mm(pg[:, 0:MB], w1[:, kc, c * 128:(c + 1) * 128], xT[:, kc, :], kc == 0, kc == 7, [w1.key, xT.key], [pg.key])
            for kc in range(8):
                kb.mm(pl[:, 0:MB], w1[:, kc, 1024 + c * 128:1024 + (c + 1) * 128], xT[:, kc, :], kc == 0, kc == 7, [w1.key, xT.key], [pl.key])
            g = gr.next(); sg = sr.next(); l = lr.next()
            kb.ts('dve', g[:], pg[:, 0:MB], b1[:, c:c + 1], 7.0, ALU.add, ALU.min, [pg.key, b1.key], [g.key])
            kb.act(sg[:], g[:], AF.Sigmoid, [g.key], [sg.key], scale=1.702)
            kb.ts('dve', l[:], pl[:, 0:MB], b1[:, 8 + c:9 + c], 7.0, ALU.add, ALU.min, [pl.key, b1.key], [l.key])
            kb.ts('dve', l[:], l[:], -7.0, 1.0, ALU.max, ALU.add, [l.key], [l.key])
            kb.tt('dve', g[:], g[:], sg[:], ALU.mult, [g.key, sg.key], [g.key])
            kb.tt('dve', aT[:, c, :], g[:], l[:], ALU.mult, [g.key, l.key], [(aT.key, c)])
        for j in range(MB // 128):
            y = yr.next()
            pa = psY.next(); pb = psY.next()
            for nh, p in enumerate((pa, pb)):
                for kc in range(8):
                    kb.mm(p[:, :], aT[:, kc, j * 128:(j + 1) * 128], w2[:, kc, nh * 512:(nh + 1) * 512], kc == 0, kc == 7,
                          [aT.key, w2.key], [p.key])
            kb.tt('dve', y[:, 0:512], pa[:, :], b2[:, 0:512], ALU.add, [pa.key, b2.key], [(y.key, 0)])
            kb.tt('dve', y[:, 512:1024], pb[:, :], b2[:, 512:1024], ALU.add, [pb.key, b2.key], [(y.key, 1)])
            kb.dma('sp', M['ys'][b * MB + j * 128:b * MB + (j + 1) * 128, :], y[:], [y.key], [('ys', b, j)])


def phase_CB(kb, L, M, mods_dram, outs, final_g=None):
    rows = {}
    for si, s in enumerate('lc'):
        t = kb.sb(f"r_{s}A2", [128, 1024], F32)
        kb.dma('sp', t[:], mods_dram[si:si + 1, 5 * 1024:6 * 1024].partition_broadcast(128), ['mods_dram'], [t.key])
        rows[s] = t
    fg = None
    if final_g is not None:
        fg = kb.sb("r_fg", [128, 1024], F32)
        kb.dma('sp', fg[:], final_g.partition_broadcast(128), [], ['r_fg'])
    sli = kb.sb("sli", [128, NTT, 4], I32)
    gk = kb.sb("gk", [128, NTT, 4], F32)
    kb.dma('sp', sli[:].rearrange("p t k -> p (t k)"), M['rt_slot'], ['rt_slot'], ['sli'])
    kb.dma('sp', gk[:].rearrange("p t k -> p (t k)"), M['rt_gk'], ['rt_gk'], ['gk'])
    gr = rings(kb, "yg", 8, [128, 1024], F32)
    accr = rings(kb, "acc", 2, [128, 1024], F32)
    xr = rings(kb, "xm", 2, [128, 1024], F32)
    xo = rings(kb, "xo", 2, [128, 1024], F32)
    rr = dict(junk=kb.sb("junk", [128, 1024], BF16), ss=rings(kb, "ss", 4, [128, 1], F32), rs=rings(kb, "rs", 4, [128, 1], F32))
    for t in range(NTT):
        is_ctx = t >= NT_OWN
        if is_ctx and outs[1] is None:
            continue
        s = 'c' if is_ctx else 'l'
        gs = []
        for k in range(4):
            g = gr.next()
            kb.P.op('pool', (lambda ga, tt_, kk: (lambda e: e.indirect_dma_start(
                out=ga[:], out_offset=None, in_=M['ys'],
                in_offset=bass.IndirectOffsetOnAxis(ap=sli[:, tt_, kk:kk + 1], axis=0))))(g, t, k),
                ['sli', 'ys'], [g.key], dma=True)
            gs.append(g)
        xm = xr.next()
        kb.dma('sp', xm[:], M['x_mid'][t * 128:(t + 1) * 128, :], ['x_mid'], [xm.key])
        acc = accr.next()
        kb.ts('dve', acc[:], gs[0][:], gk[:, t, 0:1], None, ALU.mult, None, [gs[0].key, 'gk'], [acc.key])
        for k in range(1, 4):
            kb.stt('dve', acc[:], gs[k][:], gk[:, t, k:k + 1], acc[:], ALU.mult, ALU.add, [gs[k].key, 'gk', acc.key], [acc.key])
        A2 = rows[s]
        kb.tt('dve', acc[:], acc[:], A2[:], ALU.mult, [acc.key, A2.key], [acc.key])
        x2 = xo.next()
        kb.tt('dve', x2[:], acc[:], xm[:], ALU.add, [acc.key, xm.key], [x2.key])
        if fg is not None and not is_ctx:
            ss = rr['ss'].next(); rs = rr['rs'].next()
            kb.act(rr['junk'][:], x2[:], AF.Square, [x2.key], ['junk', ss.key], accum=ss[:])
            kb.ts('dve', rs[:], ss[:], 1.0 / 1024, EPS, ALU.mult, ALU.add, [ss.key], [rs.key])
            kb.act(rs[:], rs[:], AF.Sqrt, [rs.key], [rs.key])
            kb.recip(rs[:], rs[:], [rs.key], [rs.key])
            kb.stt('dve', x2[:], x2[:], rs[:], fg[:], ALU.mult, ALU.mult, [x2.key, rs.key, 'r_fg'], [x2.key])
        dst = outs[1][(t - NT_OWN) * 128:(t - NT_OWN + 1) * 128, :] if is_ctx else outs[0][t * 128:(t + 1) * 128, :]
        kb.dma('sp', dst, x2[:], [x2.key], [('xout', t)])


def even_scratch(nc):
    kind = "Internal"
    return dict(qaT=nc.dram_tensor("qaT", [8, 64, NQA], BF16, kind=kind).ap(),
                kaT=nc.dram_tensor("kaT", [2, 64, NKA], BF16, kind=kind).ap(),
                va=nc.dram_tensor("va", [NKA, 128], BF16, kind=kind).ap(),
                qmT=nc.dram_tensor("qmT", [8, 96, NQA], BF16, kind=kind).ap(),
                kmT=nc.dram_tensor("kmT", [8, 96, NKM], BF16, kind=kind).ap(),
                vm=nc.dram_tensor("vm", [NKM, 512], BF16, kind=kind).ap(),
                oT=nc.dram_tensor("oT", [16, 64, NQA], BF16, kind=kind).ap())


def decl_inputs(nc, even):
    def din(name, shape, dt=F32):
        return nc.dram_tensor(name, shape, dt, kind="ExternalInput").ap()
    L = dict(cvec=din("cvec", [128, 16]), ada_w=din("ada_w", [1024, 6144]), ada_b=din("ada_b", [1, 6144]),
             n1g=din("n1g", [1, 1024]), n2g=din("n2g", [1, 1024]),
             x_own=din("x_own", [NQ, 1024]), x_oth=din("x_oth", [NO, 1024]), x_ctx=din("x_ctx", [NCX, 1024]),
             w_out=din("w_out", [1024, 1024]),
             router_w=din("router_w", [1024, 32]), router_b=din("router_b", [1, 32]),
             w1=din("w1", [32 * 1024, 2048]), b1=din("b1", [32 * 128, 16]), w2=din("w2", [32 * 1024, 1024]), b2=din("b2", [32, 1024]),
             final_g=din("final_g", [1, 1024]))
    if not even:
        L.update(w_in_odd=din("w_in_odd", [1024, 3072]), nab_g=din("nab_g", [128, 16, 256]), nab_s=din("nab_s", [7, 128, 16, 384]))
    if even:
        L.update(w_in_ext=din("w_in_ext", [1024, W_IN_EXT]), w_uq_ext=din("w_uq_ext", [768, 1536]), w_ukv=din("w_ukv", [256, 1024]),
                 qng=din("qng", [1, 768]), kvng=din("kvng", [1, 256]),
                 tab64=din("tab64", [64, 2, NKM]), tab96=din("tab96", [96, 2, NKM]),
                 wmask=din("wmask", [128, 4, 512]), sinkb=din("sinkb", [64, 2, 512]))
    return L


def build_layer(nc, L, even, last, outs, stop_after=None):
    mods = nc.dram_tensor("mods", [2, 6144], F32, kind="Internal").ap()
    M = moe_scratch(nc)
    with Phase(nc, 'mods') as kb:
        phase_mods(kb, L, mods)
    if even:
        S = even_scratch(nc)
        with Phase(nc, 'E1') as kb:
            phase_E1(kb, L, S, mods)
        with Phase(nc, 'E2a') as kb:
            phase_E2a(kb, L, S)
        with Phase(nc, 'E2b') as kb:
            phase_E2b(kb, L, S)
    else:
        S = odd_scratch(nc)
        with Phase(nc, 'O1') as kb:
            phase_O1(kb, L, S, mods)
        with Phase(nc, 'O2') as kb:
            phase_O2(kb, L, S)
    with Phase(nc, 'OP') as kb:
        phase_OP(kb, L, S, M, mods)
    if stop_after == 'OP':
        return M
    with Phase(nc, 'MX') as kb:
        phase_MX(kb, L, M)
    if os.environ.get('NO_CB'):
        return M
    with Phase(nc, 'CB') as kb:
        phase_CB(kb, L, M, mods, outs, final_g=L['final_g'] if last else None)
    return M


NROWP = 72
NKN = NROWP * 64 + NCX
CTXK = NROWP * 64


def odd_scratch(nc):
    kind = "Internal"
    return dict(qT=nc.dram_tensor("qTn", [16, 64, NQA], BF16, kind=kind).ap(),
                kT=nc.dram_tensor("kTn", [16, 64, NKN], BF16, kind=kind).ap(),
                v=nc.dram_tensor("vn", [NKN, 1024], BF16, kind=kind).ap(),
                oT=nc.dram_tensor("oTn", [16, 64, NQA], BF16, kind=kind).ap())


def phase_O1(kb, L, S, mods_dram):
    C = {}
    C['ident'], _ = make_ident(kb)
    g1 = kb.sb("r_g1", [128, 1024], F32)
    kb.dma('sp', g1[:], L['n1g'].partition_broadcast(128), [], ['r_g1'])
    rows = {}
    for si, s in enumerate('lc'):
        tS = kb.sb(f"r_{s}S1", [128, 1024], F32)
        tG = kb.sb(f"r_{s}G1", [128, 1024], F32)
        kb.dma('sp', tS[:], mods_dram[si:si + 1, 0:1024].partition_broadcast(128), ['mods_dram'], [tS.key])
        kb.dma('sp', tG[:], mods_dram[si:si + 1, 1024:2048].partition_broadcast(128), ['mods_dram'], [tG.key])
        kb.stt('dve', tG[:], tG[:], 1.0, g1[:], ALU.add, ALU.mult, [tG.key, 'r_g1'], [tG.key])
        rows[s + 'S1'] = tS
        rows[s + 'G1'] = tG
    win = kb.sb("w_in_o", [128, 8, 3072], BF16)
    for kc in range(8):
        kb.dma('pool', win[:, kc, :], L['w_in_odd'][kc * 128:(kc + 1) * 128, :], [], [('w_in_o', kc)])
    C['psT'] = rings(kb, "psT", 2, [128, 1024], BF16, psum=True)
    psG = rings(kb, "psG", 4, [128, 512], F32, psum=True)
    rr = dict(junk=kb.sb("junk", [128, 1024], BF16), ss=rings(kb, "ss", 4, [128, 1], F32),
              rs=rings(kb, "rs", 4, [128, 1], F32), h32=rings(kb, "h32_", 2, [128, 1024], F32))
    xr = rings(kb, "xt", 2, [128, 1024], F32)
    hbr = rings(kb, "hb", 2, [128, 1024], BF16)
    hTr = rings(kb, "hT", 2, [128, 8, 512], BF16)
    st_q = kb.sb("st_q", [64, 16, 512], BF16)
    st_k = kb.sb("st_k", [64, 16, 512], BF16)
    st_v = kb.sb("st_v", [128, 4, 1024], BF16)
    zt = kb.sb("zt", [128, 1024], BF16)
    kb.memset('pool', zt[:], 0.0, ['zt'])
    kb.dma('sp', S['kT'][:, :, CTXK - 64:CTXK].rearrange("h d t -> d h t"), zt[0:64, 0:1024].rearrange("p (h t) -> p h t", h=16), ['zt'], [('kTn', 'pad')])
    kb.dma('sp', S['v'][CTXK - 64:CTXK, :], zt[0:64, :], ['zt'], [('vn', 'pad')])

    def block(setname, blk, bw, do_q, kdst):
        s = 'c' if setname == 'ctx' else 'l'
        xsrc = L['x_' + setname]
        nt = bw // 128
        hT = hTr.next()
        for j in range(nt):
            xt = xr.next()
            r0 = blk * 512 + j * 128
            kb.dma('sp', xt[:], xsrc[r0:r0 + 128, :], [], [xt.key])
            hb = hbr.next()
            C['_outkey'] = hb.key
            norm_tile(kb, C, (xt[:], [xt.key]), (rows[s + 'G1'][:], [rows[s + 'G1'].key]),
                      (rows[s + 'S1'][:], [rows[s + 'S1'].key]), 1024, rr, hb[:])
            transpose_to(kb, C, hb, hb.key, 8, hT[:, :, j * 128:(j + 1) * 128], (hT.key, j), eng='act' if j % 2 == 0 else 'dve')
        hk = [hT.key, 'w_in_o']
        if do_q:
            for h in range(16):
                g = psG.next()
                proj_fm(kb, g, 64, win, h * 64, 8, hT, bw, hk)
                kb.copy('act' if h % 2 == 0 else 'dve', st_q[:, h, 0:bw], g[0:64, 0:bw], [g.key], [('st_q', h)])
            qc = NQ if setname == 'ctx' else blk * 512
            kb.dma('sp', S['qT'][:, :, qc:qc + bw].rearrange("h d t -> d h t"), st_q[:, :, 0:bw], ['st_q'], [('qTn', setname, blk)])
        for h in range(16):
            g = psG.next()
            proj_fm(kb, g, 64, win, 1024 + h * 64, 8, hT, bw, hk)
            kb.copy('act' if h % 2 == 1 else 'dve', st_k[:, h, 0:bw], g[0:64, 0:bw], [g.key], [('st_k', h)])
        for j in range(nt):
            for nh in range(2):
                g = psG.next()
                for kc in range(8):
                    kb.mm(g[:, :], hT[:, kc, j * 128:(j + 1) * 128], win[:, kc, 2048 + nh * 512:2048 + (nh + 1) * 512], kc == 0, kc == 7, hk, [g.key])
                kb.copy('act' if nh == 0 else 'dve', st_v[:, j, nh * 512:(nh + 1) * 512], g[:, :], [g.key], [('st_v', j, nh)])
        for (sc, ncol, dc) in kdst:
            kb.dma('sp', S['kT'][:, :, dc:dc + ncol].rearrange("h d t -> d h t"), st_k[:, :, sc:sc + ncol], ['st_k'], [('kTn', dc)])
            for o in range(0, ncol, 64):
                j, po = divmod(sc + o, 128)
                kb.dma('sp', S['v'][dc + o:dc + o + 64, :], st_v[po:po + 64, j, :], ['st_v'], [('vn', dc + o)])

    for blk in range(NQ // 512):
        block('own', blk, 512, True, [(0, 512, 256 + blk * 512)])
    block('oth', NO // 512 - 1, 512, False, [(256, 256, 0)])
    block('oth', 0, 512, False, [(0, 192, 256 + NQ)])
    block('ctx', 0, 256, True, [(0, 256, CTXK)])


def phase_O2(kb, L, S):
    C = {}
    attn_common(kb, C)
    scale = 64 ** -0.5
    esf = rings(kb, "esf", 1, [128, 16, 384], F32)
    egf = esf.tiles[0]
    eg = kb.sb("eg", [128, 16, 256], BF16)
    kb.dma('sp', egf[:, :, 0:256], L['nab_g'], [], [egf.key])
    kb.act(eg[:], egf[:, :, 0:256], AF.Exp, [egf.key], ['eg'])
    esr = rings(kb, "es", 1, [128, 16, 384], BF16)
    kcx = kb.sb("kctx", [64, 16, NCX], BF16)
    kb.dma('sp', kcx[:], S['kT'][:, :, CTXK:CTXK + NCX].rearrange("h d t -> d h t"), ['kTn'], ['kctx'])
    vcx = kb.sb("vctx", [128, 2, 16, 128], BF16)
    kb.memset('pool', vcx[:], 1.0, ['vctx'])
    for t in range(2):
        kb.dma('sp', vcx[:, t, :, 0:64], S['v'][CTXK + t * 128:CTXK + (t + 1) * 128, :].rearrange("p (h d) -> p h d", h=16), ['vn'], [('vctx', t)])
    kwr = rings(kb, "kwin", 2, [64, 16, 768], BF16)
    vwr = rings(kb, "vwin", 2, [128, 6, 16, 128], BF16)
    for v in vwr.tiles:
        kb.memset('pool', v[:], 1.0, [v.key])
    QR = 4
    qr = rings(kb, "qn_blk", 2, [64, 16, QR * 64], BF16)
    so = rings(kb, "so_n", 1, [64, 16, QR * 64], BF16)
    special = {0: 0, 1: 1, 2: 2, 3: 3, 61: 4, 62: 5, 63: 6}
    pend_pv = [None]

    def flush_pv():
        if pend_pv[0] is not None:
            pend_pv[0]()
            pend_pv[0] = None

    q = None
    o = None
    ops = None
    for rl in range(64):
        if rl % QR == 0:
            q = qr.next(); o = so.next()
            kb.dma('sp', q[:], S['qT'][:, :, rl * 64:(rl + QR) * 64].rearrange("h d t -> d h t"), ['qTn'], [q.key])
        if rl in special:
            ng = 6
            start = 0 if rl < 4 else 59
            et = esr.next(); ef = esf.next()
            kb.dma('sp', ef[:], L['nab_s'][special[rl]], [], [ef.key])
            kb.act(et[:], ef[:], AF.Exp, [ef.key], [et.key])
            ebias = et
        else:
            ng = 4
            start = rl
            ebias = eg
        kw = kwr.next(); vw = vwr.next()
        c0 = start * 64
        kb.dma('sp', kw[:, :, 0:ng * 128], S['kT'][:, :, c0:c0 + ng * 128].rearrange("h d t -> d h t"), ['kTn'], [kw.key])
        for ti in range(ng):
            kb.dma('sp', vw[:, ti, :, 0:64], S['v'][c0 + ti * 128:c0 + (ti + 1) * 128, :].rearrange("p (h d) -> p h d", h=16), ['vn'], [(vw.key, ti)])
        nt = ng + 2
        for h in range(16):
            hh = h % 8
            if hh == 0:
                ops_new = C['psO'].next()
            pss = C['psS'].next()
            pt = C['pT'].next()
            qrhs = q[:, h, (rl % QR) * 64:(rl % QR + 1) * 64]
            for ti in range(ng):
                kb.mm(pss[:, ti * 64:(ti + 1) * 64], kw[:, h, ti * 128:(ti + 1) * 128], qrhs, True, True, [kw.key, q.key], [(pss.key, ti)])
            for ti in range(2):
                kb.mm(pss[:, (ng + ti) * 64:(ng + ti + 1) * 64], kcx[:, h, ti * 128:(ti + 1) * 128], qrhs, True, True, ['kctx', q.key], [(pss.key, ng + ti)])
            kb.act(pt[:, 0:nt * 64], pss[:, 0:nt * 64], AF.Exp, [pss.key], [pt.key], scale=scale)
            kb.tt('pool', pt[:, 0:ng * 64], pt[:, 0:ng * 64], ebias[:, h, 0:ng * 64], ALU.mult, [pt.key, ebias.key], [pt.key])
            flush_pv()
            if hh == 0:
                if C.get('pending') is not None:
                    C['pending']()
                    C['pending'] = None
                ops = ops_new

            def pv(ops=ops, pt=pt, vw=vw, h=h, hh=hh, ng=ng, o=o, rl=rl):
                for ti in range(ng):
                    kb.mm(ops[:, hh * 64:(hh + 1) * 64], vw[:, ti, h, :], pt[:, ti * 64:(ti + 1) * 64], ti == 0, False, [vw.key, pt.key], [(ops.key, hh)])
                for ti in range(2):
                    kb.mm(ops[:, hh * 64:(hh + 1) * 64], vcx[:, ti, h, :], pt[:, (ng + ti) * 64:(ng + ti + 1) * 64], False, ti == 1, ['vctx', pt.key], [(ops.key, hh)])
                if hh == 7:
                    g0 = h - 7
                    C['pending'] = lambda: attn_finalize(kb, C, ops, 512, None, o[:, g0:g0 + 8, (rl % QR) * 64:(rl % QR + 1) * 64], (o.key, rl % QR, g0), split=8)
            pend_pv[0] = pv
        if rl % QR == QR - 1:
            flush_pv()
            attn_flush(kb, C)
            kb.dma('sp', S['oT'][:, :, (rl - QR + 1) * 64:(rl + 1) * 64].rearrange("h d t -> d h t"), o[:], [o.key], [('oTn', rl // QR)])
    qc = qr.next(); oc = so.next()
    kb.dma('sp', qc[:, :, 0:NCX], S['qT'][:, :, NQ:NQ + NCX].rearrange("h d t -> d h t"), ['qTn'], [qc.key])
    for h in range(16):
        tl = [(kcx[:, h, t * 128:(t + 1) * 128], ['kctx'], vcx[:, t, h, :], ['vctx'], None) for t in range(2)]
        attn_unit(kb, C, qc[:, h, 0:NCX], [qc.key], NCX, tl, scale, None, oc[:, h, 0:NCX], (oc.key, h))
    attn_flush(kb, C)
    kb.dma('sp', S['oT'][:, :, NQ:NQ + NCX].rearrange("h d t -> d h t"), oc[:, :, 0:NCX], [oc.key], [('oTn', 'c')])


GRID_W = 64
def rope_tabs(pos_rows, pos_cols, is_ctx):
    T = len(pos_rows)
    def tab(n):
        h = n // 2; half = h // 2
        d = np.arange(n)
        grp = d // h
        i = (d % h) % half
        inv = 10000.0 ** (-(i.astype(np.float32) / half))
        pos = np.where(grp[:, None] == 0, pos_rows[None, :], pos_cols[None, :]).astype(np.float32)
        ang = pos * inv[:, None].astype(np.float32)
        cos = np.cos(ang).astype(np.float32); sin = np.sin(ang).astype(np.float32)
        sign = np.where((d % h) < half, -1.0, 1.0).astype(np.float32)
        sin = sin * sign[:, None]
        cos[:, is_ctx] = 1.0; sin[:, is_ctx] = 0.0
        return cos, sin
    c64, s64 = tab(64)
    c32, s32 = tab(32)
    tab64 = np.stack([c64, s64], axis=1)
    tab96 = np.zeros((96, 2, T), np.float32)
    tab96[:64, 0] = 1.0
    tab96[64:, 0] = c32; tab96[64:, 1] = s32
    return np.ascontiguousarray(tab64), np.ascontiguousarray(tab96)

def partner(n):
    h = n // 2; half = h // 2
    d = np.arange(n)
    return np.where((d % h) < half, d + half, d - half)

def prep_even(w_in, w_uq):
    p64 = partner(64); p32 = partner(32)
    qa = w_in[:, 0:512].reshape(1024, 8, 64)[:, :, p64].reshape(1024, 512)
    ka = w_in[:, 512:640].reshape(1024, 2, 64)[:, :, p64].reshape(1024, 128)
    kpe = w_in[:, 1792:1824][:, p32]
    w_in_ext = np.ascontiguousarray(np.concatenate([w_in, qa, ka, kpe], axis=1))
    wq = w_uq.reshape(768, 8, 96).copy()
    wq[:, :, 64:] = wq[:, :, 64:][:, :, p32]
    w_uq_ext = np.ascontiguousarray(np.concatenate([w_uq, wq.reshape(768, 768)], axis=1))
    return w_in_ext, w_uq_ext

def core_positions(half):
    own = np.arange(4096) + half * 4096
    oth = np.arange(4096) + (1 - half) * 4096
    t = np.concatenate([own, oth, np.zeros(256, np.int64)])
    is_ctx = np.concatenate([np.zeros(8192, bool), np.ones(256, bool)])
    return t // GRID_W, t % GRID_W, is_ctx


def win_consts(half, sink):
    j = np.arange(128)[:, None]; i = np.arange(128)[None, :]
    ge = (j >= i).astype(np.float32); le = (j <= i).astype(np.float32)
    fp = 1.0 if half == 1 else 0.0
    fn = 1.0 if half == 0 else 0.0
    m = np.stack([ge, le, ge * fp, le * fn], axis=1)
    wmask = np.ascontiguousarray(np.tile(m[:, :, None, :], (1, 1, 4, 1)).reshape(128, 4, 512).astype(np.float32))
    sb = np.broadcast_to(sink.reshape(1, 2, 4, 1), (64, 2, 4, 128)).reshape(64, 2, 512)
    return wmask, np.ascontiguousarray(sb.astype(np.float32))


def even_inputs(d, b, half, layer, x_full=None, xc=None):
    i = layer // 2
    c = d['c'][b]; cc = d['c_ctx']
    cvec = np.ascontiguousarray(np.concatenate([c.reshape(8, 128).T, cc.reshape(8, 128).T], axis=1).astype(np.float32))
    w_in_ext, w_uq_ext = prep_even(d['ev_w_in'][i], d['ev_w_uq'][i])
    pr, pc, isc = core_positions(half)
    tab64, tab96 = rope_tabs(pr, pc, isc)
    x = d['x'][b] if x_full is None else x_full
    xc = d['ctx'][b] if xc is None else xc
    wmask, sinkb = win_consts(half, d['ev_sink'][i])
    return dict(cvec=cvec, ada_w=d['ada_w'][layer], ada_b=d['ada_b'][layer][None], n1g=d['norm1_g'][layer][None], n2g=d['norm2_g'][layer][None],
                x_own=np.ascontiguousarray(x[half * 4096:(half + 1) * 4096]), x_oth=np.ascontiguousarray(x[(1 - half) * 4096:(2 - half) * 4096]), x_ctx=xc,
                w_in_ext=w_in_ext, w_uq_ext=w_uq_ext, w_ukv=d['ev_w_ukv'][i], qng=d['ev_q_norm_g'][i][None], kvng=d['ev_kv_norm_g'][i][None],
                tab64=tab64, tab96=tab96, wmask=wmask, sinkb=sinkb, w_out=d['ev_w_out'][i])


def moe_inputs(d, layer):
    b1 = d['exp_b1'][layer]
    b1_fm = np.ascontiguousarray(b1.reshape(32, 16, 128).transpose(0, 2, 1).reshape(32 * 128, 16))
    return dict(router_w=d['router_w'][layer], router_b=d['router_b'][layer][None],
                w1=d['exp_w1'][layer].reshape(32 * 1024, 2048), b1=b1_fm,
                w2=d['exp_w2'][layer].reshape(32 * 1024, 1024), b2=d['exp_b2'][layer],
                final_g=d['final_g'][None])


NEG = -30000.0
def nab_tables(rpb, half):
    kc = np.arange(64)[:, None]; qc = np.arange(64)[None, :]
    c0 = np.clip(qc - 8, 0, 48)
    valid = (kc >= c0) & (kc <= c0 + 15)
    dc = np.clip(kc - qc + 15, 0, 30)
    T = np.where(valid[None, None], rpb[:, :, dc], NEG).astype(np.float32)
    g = np.full((128, 16, 256), NEG, np.float32)
    for i in range(8):
        g[(i % 2) * 64:(i % 2) * 64 + 64, :, (i // 2) * 64:(i // 2) * 64 + 64] = T[:, i + 3].transpose(1, 0, 2)
    s = np.full((7, 128, 16, 384), NEG, np.float32)
    for ty, rl in enumerate([0, 1, 2, 3, 61, 62, 63]):
        start = 0 if rl < 4 else 59
        r = 64 * half + rl
        r0 = min(max(r - 4, 0), 120)
        for i in range(8):
            gr = r0 + i
            w = (gr - 64 * half) + 4 - start
            assert 0 <= w < 12
            dr = gr - r + 7
            s[ty, (w % 2) * 64:(w % 2) * 64 + 64, :, (w // 2) * 64:(w // 2) * 64 + 64] = T[:, dr].transpose(1, 0, 2)
    return g, s


def odd_inputs(d, b, half, layer, x_full, xc):
    i = layer // 2
    c = d['c'][b]; cc = d['c_ctx']
    cvec = np.ascontiguousarray(np.concatenate([c.reshape(8, 128).T, cc.reshape(8, 128).T], axis=1).astype(np.float32))
    g, s = nab_tables(d['od_rpb'][i], half)
    return dict(cvec=cvec, ada_w=d['ada_w'][layer], ada_b=d['ada_b'][layer][None], n1g=d['norm1_g'][layer][None], n2g=d['norm2_g'][layer][None],
                x_own=np.ascontiguousarray(x_full[half * 4096:(half + 1) * 4096]), x_oth=np.ascontiguousarray(x_full[(1 - half) * 4096:(2 - half) * 4096]), x_ctx=xc,
                w_in_odd=d['od_w_in'][i], nab_g=g, nab_s=s, w_out=d['od_w_out'][i])


_PROG_CACHE = {}


def _build_program(even, last):
    key = (even, last)
    if key in _PROG_CACHE:
        return _PROG_CACHE[key]
    nc = bass.Bass("TRN2", target_bir_lowering=False)
    L = decl_inputs(nc, even)
    xo = nc.dram_tensor("x_out", [NQ, 1024], F32, kind="ExternalOutput").ap()
    xco = None if last else nc.dram_tensor("xc_out", [NCX, 1024], F32, kind="ExternalOutput").ap()
    build_layer(nc, L, even, last, (xo, xco))
    _PROG_CACHE[key] = nc
    return nc


def kernel(**inputs):
    d = {k: np.asarray(v) for k, v in inputs.items()}
    B = 4
    x = [np.ascontiguousarray(d['x'][b]) for b in range(B)]
    xc = [np.ascontiguousarray(d['ctx'][b]) for b in range(B)]
    for layer in range(4):
        even = layer % 2 == 0
        last = layer == 3
        nc = _build_program(even, last)
        moe = moe_inputs(d, layer)
        in_maps = []
        for core in range(8):
            b, half = core // 2, core % 2
            if even:
                m = even_inputs(d, b, half, layer, x[b], xc[b])
            else:
                m = odd_inputs(d, b, half, layer, x[b], xc[b])
            m.update(moe)
            m = {k: np.ascontiguousarray(v, dtype=np.int32 if v.dtype == np.int32 else np.float32) for k, v in m.items()}
            in_maps.append(m)
        t0 = time.time()
        res = run_bass_kernel_spmd(nc, in_maps, core_ids=list(range(8)))
        print(f'[kernel] layer {layer} launch took {time.time() - t0:.1f}s', flush=True)
        outs = res.results
        x = [np.concatenate([np.asarray(outs[2 * b]['x_out']), np.asarray(outs[2 * b + 1]['x_out'])], axis=0) for b in range(B)]
        if not last:
            xc = [np.asarray(outs[2 * b]['xc_out']) for b in range(B)]
    return np.stack(x, axis=0).astype(np.float32)
```
